# Optimizing a Trainium2 kernel written in Bass

```python
import math
import jax
import jax.numpy as jnp
from jax import lax
import numpy as np

D_MODEL = 1024
BATCH = 4
SEQ = 4096
DEPTH = 2

EPS = 1e-6
CONV_K = 4

SSD_WIDTH = D_MODEL
SSD_HEAD_DIM = 64
SSD_HEADS = SSD_WIDTH // SSD_HEAD_DIM
SSD_GROUPS = 2
SSD_STATE = 128
SSD_CHUNK = 128
SSD_CONV_DIM = SSD_WIDTH + 2 * SSD_GROUPS * SSD_STATE

ML_WIDTH = D_MODEL // 2
ML_HEADS = 4
ML_HEAD_DIM = ML_WIDTH // ML_HEADS
ML_CHUNK = 128

HG_WIDTH = D_MODEL // 2
HG_HEADS = 4
HG_HEAD_DIM = HG_WIDTH // HG_HEADS
HG_CHUNK = 16

IN_SPLITS = (SSD_WIDTH, SSD_GROUPS * SSD_STATE, SSD_GROUPS * SSD_STATE, SSD_HEADS, SSD_WIDTH,
             ML_WIDTH, ML_WIDTH, ML_WIDTH,
             HG_WIDTH, HG_WIDTH, HG_WIDTH, HG_WIDTH,
             D_MODEL, D_MODEL, D_MODEL)
N_IN = sum(IN_SPLITS)

kernel_name = 'hybrid_ssd_mlstm_hgrn2'


def rmsnorm(x, w):
    xf = x.astype(jnp.float32)
    y = xf * lax.rsqrt(jnp.mean(xf * xf, -1, keepdims=True) + EPS)
    return (y * w).astype(x.dtype)


def grouped_rmsnorm(x, w, groups):
    sh = x.shape
    xf = x.astype(jnp.float32).reshape(*sh[:-1], groups, sh[-1] // groups)
    y = xf * lax.rsqrt(jnp.mean(xf * xf, -1, keepdims=True) + EPS)
    return y.reshape(sh) * w


def head_layernorm(x, w):
    xf = x.astype(jnp.float32)
    mu = jnp.mean(xf, -1, keepdims=True)
    xc = xf - mu
    y = xc * lax.rsqrt(jnp.mean(xc * xc, -1, keepdims=True) + EPS)
    b, t, h, d = x.shape
    return y.reshape(b, t, h * d) * w


def causal_dwconv(x, w, bias):
    k = w.shape[0]
    y = lax.conv_general_dilated(x, w[:, None, :], window_strides=(1,), padding=[(k - 1, 0)],
                                 dimension_numbers=('NWC', 'WIO', 'NWC'),
                                 feature_group_count=x.shape[-1])
    return y + bias


def causal_log_decay(a):
    cs = jnp.cumsum(a, -1)
    n = a.shape[-1]
    mask = jnp.tril(jnp.ones((n, n), dtype=bool))
    return jnp.where(mask, cs[..., :, None] - cs[..., None, :], -jnp.inf)


def scan_chunk_states(decay, local):
    def step(s, inp):
        d, l = inp
        return d * s + l, s
    _, s_in = lax.scan(step, jnp.zeros_like(local[0]), (decay, local))
    return s_in


def ssd_scan(xs, bm, cm, dt_raw, dt_bias, a_log, d_skip):
    f32 = jnp.float32
    b, t, h, p = xs.shape
    g, n = bm.shape[2], bm.shape[3]
    hg = h // g
    nc = t // SSD_CHUNK
    dt = jax.nn.softplus(dt_raw.astype(f32) + dt_bias.astype(f32))
    a = -jnp.exp(a_log.astype(f32))
    la = (dt * a).reshape(b, nc, SSD_CHUNK, g, hg).transpose(0, 3, 4, 1, 2)
    cum = jnp.cumsum(la, -1)
    x_dt = (xs.astype(f32) * dt[..., None]).reshape(b, nc, SSD_CHUNK, g, hg, p)
    bc = bm.astype(f32).reshape(b, nc, SSD_CHUNK, g, n)
    cc = cm.astype(f32).reshape(b, nc, SSD_CHUNK, g, n)
    cb = jnp.einsum('bclgn,bcsgn->bgcls', cc, bc)
    scores = cb[:, :, None] * jnp.exp(causal_log_decay(la))
    y_diag = jnp.einsum('bghcls,bcsghp->bclghp', scores, x_dt)
    w_end = jnp.exp(cum[..., -1:] - cum)
    local = jnp.einsum('bcsgn,bghcs,bcsghp->cbghpn', bc, w_end, x_dt)
    chunk_decay = jnp.exp(cum[..., -1]).transpose(3, 0, 1, 2)[..., None, None]
    s_in = scan_chunk_states(chunk_decay, local)
    y_off = jnp.einsum('bclgn,cbghpn,bghcl->bclghp', cc, s_in, jnp.exp(cum))
    y = (y_diag + y_off).reshape(b, t, h, p)
    return y + xs.astype(f32) * d_skip.astype(f32)[:, None]


def mlstm_scan(q, k, v, i_pre, f_pre):
    f32 = jnp.float32
    b, t, h, d = q.shape
    L = ML_CHUNK
    nc = t // L
    qc = (q.astype(f32) * (d ** -0.5)).reshape(b, nc, L, h, d)
    kc = k.astype(f32).reshape(b, nc, L, h, d)
    vc = v.astype(f32).reshape(b, nc, L, h, d)
    li = i_pre.astype(f32).reshape(b, nc, L, h).transpose(0, 3, 1, 2)
    lf = jax.nn.log_sigmoid(f_pre.astype(f32)).reshape(b, nc, L, h).transpose(0, 3, 1, 2)
    cum = jnp.cumsum(lf, -1)
    log_d = causal_log_decay(lf) + li[..., None, :]
    log_end = cum[..., -1:] - cum + li
    m_loc = jnp.max(log_end, -1)
    w_end = jnp.exp(log_end - m_loc[..., None])
    c_loc = jnp.einsum('bhcs,bcshd,bcshe->cbhde', w_end, vc, kc)
    n_loc = jnp.einsum('bhcs,bcshe->cbhe', w_end, kc)
    g_end = cum[..., -1].transpose(2, 0, 1)
    m_loc_c = m_loc.transpose(2, 0, 1)

    def step(carry, inp):
        c_s, n_s, m_s = carry
        g, ml, cl, nl = inp
        m_new = jnp.maximum(g + m_s, ml)
        a_old = jnp.exp(g + m_s - m_new)
        a_loc = jnp.exp(ml - m_new)
        c_new = a_old[..., None, None] * c_s + a_loc[..., None, None] * cl
        n_new = a_old[..., None] * n_s + a_loc[..., None] * nl
        return (c_new, n_new, m_new), (c_s, n_s, m_s)

    init = (jnp.zeros((b, h, d, d), f32), jnp.zeros((b, h, d), f32), jnp.zeros((b, h), f32))
    _, (c_in, n_in, m_in) = lax.scan(step, init, (g_end, m_loc_c, c_loc, n_loc))
    m_in = m_in.transpose(1, 2, 0)
    log_inter = cum + m_in[..., None]
    m_t = jnp.maximum(log_inter, jnp.max(log_d, -1))
    w_intra = jnp.exp(log_d - m_t[..., None])
    w_inter = jnp.exp(log_inter - m_t)
    s = jnp.einsum('bclhe,bcshe->bhcls', qc, kc) * w_intra
    num = (jnp.einsum('bhcls,bcshd->bclhd', s, vc)
           + jnp.einsum('bclhe,cbhde,bhcl->bclhd', qc, c_in, w_inter))
    den = jnp.sum(s, -1) + jnp.einsum('bclhe,cbhe,bhcl->bhcl', qc, n_in, w_inter)
    den = jnp.maximum(jnp.abs(den), jnp.exp(-m_t))
    out = num / den.transpose(0, 2, 3, 1)[..., None]
    return out.reshape(b, t, h, d)


def hgrn2_scan(q, k, v, log_f):
    f32 = jnp.float32
    b, t, h, dk = q.shape
    dv = v.shape[-1]
    L = HG_CHUNK
    nc = t // L
    qc = q.astype(f32).reshape(b, nc, L, h, dk)
    kc = k.astype(f32).reshape(b, nc, L, h, dk)
    vc = v.astype(f32).reshape(b, nc, L, h, dv)
    cum = jnp.cumsum(log_f.astype(f32).reshape(b, nc, L, h, dk), axis=2)
    mask = jnp.tril(jnp.ones((L, L), dtype=bool))[None, None, :, :, None, None]
    diff = cum[:, :, :, None] - cum[:, :, None, :]
    dec = jnp.exp(jnp.where(mask, diff, -jnp.inf))
    attn = jnp.einsum('bclhk,bcshk,bclshk->bchls', qc, kc, dec)
    y_intra = jnp.einsum('bchls,bcshv->bclhv', attn, vc)
    w_end = jnp.exp(cum[:, :, -1:] - cum)
    local = jnp.einsum('bcshk,bcshv->cbhkv', kc * w_end, vc)
    chunk_decay = jnp.exp(cum[:, :, -1]).transpose(1, 0, 2, 3)[..., None]
    s_in = scan_chunk_states(chunk_decay, local)
    y_inter = jnp.einsum('bclhk,cbhkv->bclhv', qc * jnp.exp(cum), s_in)
    return (y_intra + y_inter).reshape(b, t, h, dv)


def hybrid_layer(x, lb, norm_w, w_in, ssd_conv_w, ssd_conv_b, ssd_dt_bias, ssd_a_log, ssd_d,
                 ssd_norm_w, ml_conv_w, ml_conv_b, ml_wq, ml_wk, ml_wv, ml_w_if, ml_b_if,
                 ml_norm_w, ml_skip, hg_norm_w, w_branch_ssd, w_branch_ml, w_branch_hg, w_out):
    f32 = jnp.float32
    b, t, _ = x.shape
    hn = rmsnorm(x, norm_w)
    proj = hn @ w_in
    split_points = [int(v) for v in np.cumsum(IN_SPLITS)[:-1]]
    (s_x, s_b, s_c, s_dt, s_z, m_x, m_o, m_z, g_q, g_f, g_i, g_z,
     gate_ssd, gate_ml, gate_hg) = jnp.split(proj, split_points, axis=-1)

    xbc = jax.nn.silu(causal_dwconv(jnp.concatenate([s_x, s_b, s_c], -1), ssd_conv_w, ssd_conv_b))
    sx, sb, sc = jnp.split(xbc, [SSD_WIDTH, SSD_WIDTH + SSD_GROUPS * SSD_STATE], axis=-1)
    y_ssd = ssd_scan(sx.reshape(b, t, SSD_HEADS, SSD_HEAD_DIM),
                     sb.reshape(b, t, SSD_GROUPS, SSD_STATE),
                     sc.reshape(b, t, SSD_GROUPS, SSD_STATE),
                     s_dt, ssd_dt_bias, ssd_a_log, ssd_d)
    y_ssd = grouped_rmsnorm(y_ssd.reshape(b, t, SSD_WIDTH) * jax.nn.silu(s_z.astype(f32)),
                            ssd_norm_w, SSD_GROUPS)

    m_conv = jax.nn.silu(causal_dwconv(m_x, ml_conv_w, ml_conv_b))
    xc_h = m_conv.reshape(b, t, ML_HEADS, ML_HEAD_DIM)
    q = jnp.einsum('bthd,hde->bthe', xc_h, ml_wq)
    k = jnp.einsum('bthd,hde->bthe', xc_h, ml_wk)
    v = jnp.einsum('bthd,hde->bthe', m_x.reshape(b, t, ML_HEADS, ML_HEAD_DIM), ml_wv)
    qkv = jnp.concatenate([q, k, v], -1).reshape(b, t, 3 * ML_WIDTH)
    if_pre = qkv @ ml_w_if + ml_b_if
    i_pre, f_pre = jnp.split(if_pre, 2, axis=-1)
    h_ml = mlstm_scan(q, k, v, i_pre, f_pre)
    y_ml = (head_layernorm(h_ml, ml_norm_w) * jax.nn.sigmoid(m_o.astype(f32))
            + ml_skip * m_conv.astype(f32))
    y_ml = y_ml * jax.nn.silu(m_z.astype(f32))

    fx = g_f.astype(f32)
    log_f = jnp.logaddexp(jax.nn.log_sigmoid(fx), jnp.log(lb) + jax.nn.log_sigmoid(-fx))
    k_hg = (1.0 - lb) * jax.nn.sigmoid(-fx)
    q_hg = jax.nn.silu(g_q.astype(f32))
    o_hg = hgrn2_scan(q_hg.reshape(b, t, HG_HEADS, HG_HEAD_DIM),
                      k_hg.reshape(b, t, HG_HEADS, HG_HEAD_DIM),
                      g_i.reshape(b, t, HG_HEADS, HG_HEAD_DIM),
                      log_f.reshape(b, t, HG_HEADS, HG_HEAD_DIM))
    y_hg = grouped_rmsnorm(o_hg.reshape(b, t, HG_WIDTH), hg_norm_w, HG_HEADS) * jax.nn.silu(g_z.astype(f32))

    dt_ = x.dtype
    merged = (jax.nn.sigmoid(gate_ssd) * (y_ssd.astype(dt_) @ w_branch_ssd)
              + jax.nn.sigmoid(gate_ml) * (y_ml.astype(dt_) @ w_branch_ml)
              + jax.nn.sigmoid(gate_hg) * (y_hg.astype(dt_) @ w_branch_hg))
    return x + (merged @ w_out).astype(dt_)


def setup_inputs(seed: int = 0) -> dict:
    key = jax.random.key(seed)
    ks = jax.random.split(key, 26)
    f32 = jnp.float32

    def nrm(k, shape, scale):
        return jax.random.normal(k, shape, f32) * scale

    def gain(k, shape):
        return 1.0 + 0.02 * jax.random.normal(k, shape, f32)

    x = nrm(ks[0], (BATCH, SEQ, D_MODEL), 1.0)
    norm_w = gain(ks[1], (DEPTH, D_MODEL))
    w_in = nrm(ks[2], (DEPTH, D_MODEL, N_IN), D_MODEL ** -0.5)
    ssd_conv_w = nrm(ks[3], (DEPTH, CONV_K, SSD_CONV_DIM), CONV_K ** -0.5)
    ssd_conv_b = nrm(ks[4], (DEPTH, SSD_CONV_DIM), 0.02)
    dt0 = jnp.exp(jax.random.uniform(ks[5], (DEPTH, SSD_HEADS), f32, math.log(1e-3), math.log(1e-1)))
    ssd_dt_bias = dt0 + jnp.log(-jnp.expm1(-dt0))
    ssd_a_log = jnp.log(jax.random.uniform(ks[6], (DEPTH, SSD_HEADS), f32, 1.0, 16.0))
    ssd_d = gain(ks[7], (DEPTH, SSD_HEADS))
    ssd_norm_w = gain(ks[8], (DEPTH, SSD_WIDTH))
    ml_conv_w = nrm(ks[9], (DEPTH, CONV_K, ML_WIDTH), CONV_K ** -0.5)
    ml_conv_b = nrm(ks[10], (DEPTH, ML_WIDTH), 0.02)
    ml_wq = nrm(ks[11], (DEPTH, ML_HEADS, ML_HEAD_DIM, ML_HEAD_DIM), ML_HEAD_DIM ** -0.5)
    ml_wk = nrm(ks[12], (DEPTH, ML_HEADS, ML_HEAD_DIM, ML_HEAD_DIM), ML_HEAD_DIM ** -0.5)
    ml_wv = nrm(ks[13], (DEPTH, ML_HEADS, ML_HEAD_DIM, ML_HEAD_DIM), ML_HEAD_DIM ** -0.5)
    ml_w_if = nrm(ks[14], (DEPTH, 3 * ML_WIDTH, 2 * ML_HEADS), (3 * ML_WIDTH) ** -0.5)
    i_bias = nrm(ks[15], (DEPTH, ML_HEADS), 0.1)
    f_bias = jnp.linspace(3.0, 6.0, ML_HEADS, dtype=f32)[None] + nrm(ks[16], (DEPTH, ML_HEADS), 0.02)
    ml_b_if = jnp.concatenate([i_bias, f_bias], -1)
    ml_norm_w = gain(ks[17], (DEPTH, ML_WIDTH))
    ml_skip = gain(ks[18], (DEPTH, ML_WIDTH))
    hg_lower_bounds = nrm(ks[19], (DEPTH, HG_WIDTH), 0.1)
    hg_norm_w = gain(ks[20], (DEPTH, HG_WIDTH))
    w_branch_ssd = nrm(ks[21], (DEPTH, SSD_WIDTH, D_MODEL), SSD_WIDTH ** -0.5)
    w_branch_ml = nrm(ks[22], (DEPTH, ML_WIDTH, D_MODEL), ML_WIDTH ** -0.5)
    w_branch_hg = nrm(ks[23], (DEPTH, HG_WIDTH, D_MODEL), HG_WIDTH ** -0.5)
    w_out = nrm(ks[24], (DEPTH, D_MODEL, D_MODEL), D_MODEL ** -0.5)
    final_norm_w = gain(ks[25], (D_MODEL,))
    return {'x': x, 'norm_w': norm_w, 'w_in': w_in, 'ssd_conv_w': ssd_conv_w,
            'ssd_conv_b': ssd_conv_b, 'ssd_dt_bias': ssd_dt_bias, 'ssd_a_log': ssd_a_log,
            'ssd_d': ssd_d, 'ssd_norm_w': ssd_norm_w, 'ml_conv_w': ml_conv_w,
            'ml_conv_b': ml_conv_b, 'ml_wq': ml_wq, 'ml_wk': ml_wk, 'ml_wv': ml_wv,
            'ml_w_if': ml_w_if, 'ml_b_if': ml_b_if, 'ml_norm_w': ml_norm_w, 'ml_skip': ml_skip,
            'hg_lower_bounds': hg_lower_bounds, 'hg_norm_w': hg_norm_w,
            'w_branch_ssd': w_branch_ssd, 'w_branch_ml': w_branch_ml, 'w_branch_hg': w_branch_hg,
            'w_out': w_out, 'final_norm_w': final_norm_w}


def reference(x, norm_w, w_in, ssd_conv_w, ssd_conv_b, ssd_dt_bias, ssd_a_log, ssd_d, ssd_norm_w,
              ml_conv_w, ml_conv_b, ml_wq, ml_wk, ml_wv, ml_w_if, ml_b_if, ml_norm_w, ml_skip,
              hg_lower_bounds, hg_norm_w, w_branch_ssd, w_branch_ml, w_branch_hg, w_out,
              final_norm_w):
    lbs = jnp.cumsum(jax.nn.softmax(hg_lower_bounds.astype(jnp.float32), axis=0), axis=0)
    lbs = lbs - lbs[0]
    for l in range(DEPTH):
        x = hybrid_layer(x, lbs[l], norm_w[l], w_in[l], ssd_conv_w[l], ssd_conv_b[l],
                         ssd_dt_bias[l], ssd_a_log[l], ssd_d[l], ssd_norm_w[l],
                         ml_conv_w[l], ml_conv_b[l], ml_wq[l], ml_wk[l], ml_wv[l],
                         ml_w_if[l], ml_b_if[l], ml_norm_w[l], ml_skip[l], hg_norm_w[l],
                         w_branch_ssd[l], w_branch_ml[l], w_branch_hg[l], w_out[l])
    return rmsnorm(x, final_norm_w)
```

```python
import numpy as np
from contextlib import ExitStack
import concourse.bass as bass
import concourse.mybir as mybir
from concourse.bass_utils import run_bass_kernel_spmd
from concourse.alu_op_type import AluOpType as ALU

AF = mybir.ActivationFunctionType
F32 = mybir.dt.float32
BF16 = mybir.dt.bfloat16

D = 1024
T = 4096
DEPTH = 2
SC = 256
NC2 = 9248
cA, cZ, cDT, cMX, cMO, cMZ, cGQ, cGF, cGZ, cGI, cGATE = 0, 1568, 1536, 2592, 3104, 3616, 4128, 4640, 5152, 5664, 6176
WGROUPS = [(0, 1536), (1536, 2592), (2592, 4128), (4128, 5664), (5664, 6176), (6176, 7712), (7712, 9248)]
HC = 64
EPS = 1e-6
NEG = -30000.0


class _Op:
    __slots__ = ("eng", "fn", "deps", "dma", "waits", "sig", "sigval", "dsem", "dval", "dprev")

    def __init__(self, eng, fn, deps, dma):
        self.eng = eng
        self.fn = fn
        self.deps = deps
        self.dma = dma
        self.waits = []
        self.sig = False
        self.sigval = 0
        self.dsem = -1
        self.dval = 0
        self.dprev = 0


class Sched:
    ENGS = ("pe", "act", "dve", "pool", "sp")

    def __init__(self, n_dma_sems=24, same_sync=True):
        self.ops = []
        self.res_w = {}
        self.res_r = {}
        self.n_dma_sems = n_dma_sems
        self.same_sync = same_sync

    def add(self, eng, fn, reads=(), writes=(), dma=False):
        deps = {}
        for r in reads:
            w = self.res_w.get(r)
            if w is not None:
                deps[w] = 2
        for r in writes:
            w = self.res_w.get(r)
            if w is not None:
                deps[w] = max(deps.get(w, 0), 1)
            rr = self.res_r.get(r)
            if rr:
                for v in rr.values():
                    deps.setdefault(v, 0)
        i = len(self.ops)
        self.ops.append(_Op(eng, fn, deps, dma))
        key = ("dma", i) if dma else eng
        for r in reads:
            self.res_r.setdefault(r, {})[key] = i
        for r in writes:
            self.res_w[r] = i
            self.res_r[r] = {}
        return i

    def finalize(self):
        ops = self.ops
        know = {e: {} for e in self.ENGS}
        know_dma = {e: {} for e in self.ENGS}
        clock = {}
        needed = set()
        for op in ops:
            needed.update(op.deps)
        dma_uses = [0] * self.n_dma_sems
        dma_rr = 0
        for i, op in enumerate(ops):
            E = op.eng
            kn = know[E]
            waits = {}
            dwaits = {}
            for d in op.deps:
                dop = ops[d]
                if dop.dma:
                    if know_dma[E].get(dop.dsem, 0) >= dop.dval:
                        continue
                    dwaits[dop.dsem] = max(dwaits.get(dop.dsem, 0), dop.dval)
                    ck = clock.get(d)
                    if ck:
                        for k, v in ck.items():
                            if kn.get(k, -1) < v:
                                kn[k] = v
                else:
                    E2 = dop.eng
                    if E2 == E and (E == "pe" or E == "sp" or not self.same_sync):
                        continue
                    if E2 == E and self.same_sync == "raw" and op.deps[d] < 2:
                        continue
                    if E2 == E and self.same_sync == "rawwaw" and op.deps[d] < 1:
                        continue
                    if kn.get(E2, -1) >= d:
                        continue
                    waits[E2] = max(waits.get(E2, -1), d)
            for E2, d in waits.items():
                if kn.get(E2, -1) >= d:
                    continue
                ck = clock[d]
                for k, v in ck.items():
                    if kn.get(k, -1) < v:
                        kn[k] = v
                ops[d].sig = True
                op.waits.append(("c", E2, d))
            for s, v in dwaits.items():
                know_dma[E][s] = v
                op.waits.append(("d", s, v))
            if op.dma:
                s = dma_rr
                dma_rr = (dma_rr + 1) % self.n_dma_sems
                op.dprev = dma_uses[s] * 16
                dma_uses[s] += 1
                op.dsem = s
                op.dval = dma_uses[s] * 16
                if op.dprev > 0 and know_dma[E].get(s, 0) < op.dprev:
                    op.waits.append(("d", s, op.dprev))
                    know_dma[E][s] = op.dprev
            if i in needed:
                ck = dict(kn)
                if not op.dma:
                    ck[E] = i
                clock[i] = ck
        cnt = {e: 0 for e in self.ENGS}
        for op in ops:
            if op.sig and not op.dma:
                cnt[op.eng] += 1
                op.sigval = cnt[op.eng]

    def emit(self, nc, sems, dsems):
        ops = self.ops
        per = {e: [] for e in self.ENGS}
        for op in ops:
            per[op.eng].append(op)

        def replay(name, eng):
            for op in per[name]:
                for w in op.waits:
                    if w[0] == "c":
                        eng.wait_ge(sems[w[1]], ops[w[2]].sigval)
                    else:
                        eng.wait_ge(dsems[w[1]], w[2])
                ins = op.fn(eng)
                if op.dma:
                    ins.then_inc(dsems[op.dsem], 16)
                elif op.sig:
                    ins.then_inc(sems[op.eng], 1)

        with nc.Block() as block:
            @block.tensor
            def _(e):
                replay("pe", e)

            @block.scalar
            def _(e):
                replay("act", e)

            @block.vector
            def _(e):
                replay("dve", e)

            @block.gpsimd
            def _(e):
                replay("pool", e)

            @block.sync
            def _(e):
                replay("sp", e)


CP = {}
_o = 0
for _n, _w in [("normw", 8), ("convbA", 12), ("convwA", 48), ("convbM", 4), ("convwM", 16), ("ssdD", 8),
               ("ssdnw", 8), ("mlnw", 4), ("mlskip", 4), ("hgnw", 4), ("hglb0", 4), ("hglb1", 4)]:
    CP[_n] = _o
    _o += _w
NCP = _o


def build_program(nsc=None, depth=DEPTH, same_sync="rawwaw"):
    W = SC
    NCH = SC // 128
    NHB = SC // HC
    if nsc is None:
        nsc = T // SC
    nc = bass.Bass("TRN2", target_bir_lowering=False)
    S = Sched(same_sync=same_sync)
    A = S.add

    def din(name, shape, dt=F32):
        return nc.dram_tensor(name, list(shape), dt, kind="ExternalInput").ap()

    x_d = din("x", [T, D])
    out_d = nc.dram_tensor("out", [T, D], F32, kind="ExternalOutput").ap()
    win_d = [din(f"win{l}", [D, NC2]) for l in range(DEPTH)]
    wb_d = [din(f"wb{l}", [2048, D]) for l in range(DEPTH)]
    wo_d = [din(f"wo{l}", [D, D]) for l in range(DEPTH)]
    mlw_d = [din(f"mlw{l}", [128, 3 * 4 * 128]) for l in range(DEPTH)]
    wif_d = [din(f"wif{l}", [128, 96]) for l in range(DEPTH)]
    cp_d = [din(f"cp{l}", [128, NCP]) for l in range(DEPTH)]
    rp16_d = [din(f"rp16_{l}", [16, 2]) for l in range(DEPTH)]
    rp4_d = [din(f"rp4_{l}", [4, 2]) for l in range(DEPTH)]
    fnw_d = din("fnw", [128, D])
    ident_d = din("ident", [128, 128])
    mask4_d = din("mask4", [128, 512])
    maskhg_d = din("maskhg", [64, 256])
    sel16_d = din("sel16", [16, 4 * 512])
    sel4_d = din("sel4", [4, 512])
    hsel_d = din("hsel", [16, 8 * 128])
    selT4_d = din("selT4", [4, 4 * 128])
    rst128_d = din("rst128", [16, 512])
    rst64_d = din("rst64", [128, 512])
    selcol_d = din("selcol", [128, 16])
    winb_d = [nc.dram_tensor(f"winb{l}", [128, 8, NC2], BF16, kind="Internal").ap() for l in range(DEPTH)]
    wbb_d = [nc.dram_tensor(f"wbb{l}", [128, 16, D], BF16, kind="Internal").ap() for l in range(DEPTH)]
    wob_d = [nc.dram_tensor(f"wob{l}", [128, 8, D], BF16, kind="Internal").ap() for l in range(DEPTH)]

    with ExitStack() as es:
        tot = [0]

        def sb(name, shape, dt):
            n = 1
            for v in shape[1:]:
                n *= v
            tot[0] += n * (2 if dt == BF16 else 4)
            return es.enter_context(nc.sbuf_tensor("s_" + name, list(shape), dt))

        def pst(name, shape, dt):
            return es.enter_context(nc.psum_tensor(name, list(shape), dt))

        NB = 6
        banks = [pst(f"pb{i}", [128, 512], F32) for i in range(NB)]
        tbanks = [pst(f"tb{i}", [128, 1024], BF16) for i in range(2)]
        bctr = [0]
        tctr = [0]

        def bank():
            i = bctr[0] % NB
            bctr[0] += 1
            return banks[i], ("pb", i)

        def tbank():
            i = tctr[0] % 2
            tctr[0] += 1
            return tbanks[i], ("tb", i)

        idf = sb("idf", [128, 128], F32)
        idb = sb("idb", [128, 128], BF16)
        onesb = sb("onesb", [128, 128], BF16)
        onesf = sb("onesf", [128, 512], F32)
        mask4b = sb("mask4b", [128, 512], BF16)
        maskhg = sb("maskhg", [64, 256], F32)
        sel16 = sb("sel16", [16, 4, 512], F32)
        sel4 = sb("sel4", [4, 512], F32)
        hsel = sb("hsel", [16, 8, 128], F32)
        selT4 = sb("selT4", [4, 4, 128], F32)
        rst128 = sb("rst128", [16, W], F32)
        rst64 = sb("rst64", [128, W], F32)
        selcolf = sb("selcolf", [128, 16], F32)
        selcolb = sb("selcolb", [128, 4, 4], BF16)

        cp = [sb(f"cp{l}", [128, NCP], F32) for l in range(depth)]
        rp16 = [sb(f"rp16_{l}", [16, 2], F32) for l in range(depth)]
        rp4 = [sb(f"rp4_{l}", [4, 2], F32) for l in range(depth)]
        negA = [sb(f"negA{l}", [16, 1], F32) for l in range(depth)]
        negA0 = [sb(f"negA0{l}", [16, 1], F32) for l in range(depth)]
        nbf = [sb(f"nbf{l}", [4, 1], F32) for l in range(depth)]
        lbc = [sb(f"lbc{l}", [128, 4], F32) for l in range(depth)]
        omlc = [sb(f"omlc{l}", [128, 4], F32) for l in range(depth)]
        mlw = [sb(f"mlw{l}", [128, 3, 4, 128], BF16) for l in range(depth)]
        wif = [sb(f"wif{l}", [128, 12, 8], BF16) for l in range(depth)]
        haloA = [sb(f"haloA{l}", [128, 12, 4], BF16) for l in range(depth)]
        haloM = [sb(f"haloM{l}", [128, 4, 4], BF16) for l in range(depth)]
        Sssd = [sb(f"Sssd{l}", [128, 1024], F32) for l in range(depth)]
        Cml = [sb(f"Cml{l}", [128, 4, 129], F32) for l in range(depth)]
        Shg = [sb(f"Shg{l}", [128, 4, 128], F32) for l in range(depth)]
        Gprev = [sb(f"Gprev{l}", [4, 1], F32) for l in range(depth)]
        Mprev = [sb(f"Mprev{l}", [4, 1], F32) for l in range(depth)]
        Sssdb = sb("Sssdb", [128, 1024], BF16)
        Cmlb = sb("Cmlb", [128, 4, 129], BF16)
        Shgb = sb("Shgb", [128, 4, 128], BF16)
        diag = sb("diag", [128, 2, 4, 128], BF16)

        NSLOT = 4
        SLW = 768
        wslot = [sb(f"wslot{i}", [128, 8, SLW], BF16) for i in range(NSLOT)]
        xres = sb("xres", [128, NCH, D], F32)
        hnT = sb("hnT", [128, 8, W], BF16)
        oR1 = 12 * (W + 4)
        oR2 = oR1 + 12 * W
        ABN = oR2 + 4 * W
        arena_b = sb("arena_b", [128, ABN], BF16)
        FW = 4 * W
        arena_f = sb("arena_f", [128, 3 * FW], F32)
        yT = sb("yT", [128, 16, W], BF16)
        mergedT = hnT
        ssq = sb("ssq", [128, 4], F32)
        rs1 = sb("rs1", [128, 4], F32)
        rs2 = sb("rs2", [128, 4], F32)
        rs3 = sb("rs3", [128, 4], F32)
        rs4 = sb("rs4", [128, 4], F32)
        xsb = sb("xsb", [128, D], BF16)
        tok32 = sb("tok32", [128, 32], F32)
        decbc = sb("decbc", [128, 16], F32)
        r16a = sb("r16a", [16, W], F32)
        r16b = sb("r16b", [16, W], F32)
        r16c = sb("r16c", [16, W], F32)
        r16d = sb("r16d", [16, W], F32)
        r16e = sb("r16e", [16, W], F32)
        r16f = sb("r16f", [16, 16], F32)
        r16g = sb("r16g", [16, 1], F32)
        r4 = {n: sb("r4" + n, [4, W], F32) for n in ["G", "R", "Mt", "nMt", "emt", "wint", "wend"]}
        r4["e1"] = r4["wint"]
        r4["lfn"] = r4["wend"]
        r4["t"] = r4["wint"]
        r4den = sb("r4den", [4, 128], F32)
        r4den2 = sb("r4den2", [4, 128], F32)
        r4rden = sb("r4rden", [4, 128], F32)
        r4s = sb("r4s", [4, 16], F32)
        aoldbc = sb("aoldbc", [128, 16], F32)
        nsel = sb("nsel", [128, 4, 4], BF16)
        xdt_t = sb("xdt_t", [128, 1024], BF16)
        xdtw_t = sb("xdtw_t", [128, 1024], BF16)
        btok_t = sb("btok_t", [128, 256], BF16)
        dec_t = sb("dec_t", [128, 512], BF16)
        sc_t = sb("sc_t", [128, 2048], BF16)
        ebc_t = sb("ebc_t", [128, 512], F32)
        ytmp_t = sb("ytmp_t", [128, 512], F32)
        sq_t = sb("sq_t", [128, 4, W], BF16)
        nrm_t = sb("nrm_t", [128, W], F32)
        nrm2_t = sb("nrm2_t", [128, W], F32)
        kw_t = sb("kw_t", [128, 4, 128], BF16)
        v1_t = sb("v1_t", [128, 4, 129], BF16)
        qw_t = sb("qw_t", [128, 512], BF16)
        hdec_t = sb("hdec_t", [128, 4, NHB], F32)
        kwtok_t = sb("kwtok_t", [64, 512], BF16)
        vtok_t = sb("vtok_t", [64, 512], BF16)
        attn_t = sb("attn_t", [64, 256], BF16)
        yg_t = yT
        sel4b = sb("sel4b", [4, 512], BF16)
        Rhi = sb("Rhi", [4, W], BF16)
        Rlo = sb("Rlo", [4, W], BF16)
        Rhif = sb("Rhif", [4, W], F32)
        nMhi = sb("nMhi", [4, W], BF16)
        nMlo = sb("nMlo", [4, W], BF16)
        nMhif = sb("nMhif", [4, W], F32)
        ndhi = sb("ndhi", [4, 512], BF16)
        ndlo = sb("ndlo", [4, 512], BF16)
        sel16b = sb("sel16b", [16, 4, 512], BF16)
        cumhi = sb("cumhi", [16, W], BF16)
        cumhif = sb("cumhif", [16, W], F32)
        cumlo = sb("cumlo", [16, W], BF16)
        ncumhi = sb("ncumhi", [16, W], BF16)
        ncumlo = sb("ncumlo", [16, W], BF16)
        cdhi = sb("cdhi", [16, 512], BF16)
        cdlo = sb("cdlo", [16, 512], BF16)
        hselb = sb("hselb", [16, 8, 128], BF16)
        selT4b = sb("selT4b", [4, 4, 128], BF16)
        ecumb = sb("ecumb", [16, W], BF16)
        wintb = sb("wintb", [4, W], BF16)
        rdenb = sb("rdenb", [4, 128], BF16)
        sqf_t = ytmp_t
        print("SBUF bytes/partition:", tot[0])

        def view(base, off, shape):
            n = 1
            for v in shape:
                n *= v
            v = base[:, off:off + n]
            if len(shape) == 2:
                return v.rearrange("p (a b) -> p a b", a=shape[0])
            return v

        ARB = ["rawA", "convA", "vT"]
        ARF = ["f0", "f1", "f2"]

        def load(dst, src, res):
            A("sp", lambda e: e.dma_start(out=dst, in_=src), writes=[res] if not isinstance(res, list) else res, dma=True)

        load(idf[:], ident_d, "idf")
        load(maskhg[:], maskhg_d, "maskhg")
        load(sel16[:], sel16_d.rearrange("p (a b) -> p a b", a=4), "sel16")
        load(sel4[:], sel4_d, "sel4")
        load(hsel[:], hsel_d.rearrange("p (a b) -> p a b", a=8), "hsel")
        load(selT4[:], selT4_d.rearrange("p (a b) -> p a b", a=4), "selT4")
        load(rst128[:], rst128_d[:, 0:W], "rst128")
        load(rst64[:], rst64_d[:, 0:W], "rst64")
        load(selcolf[:], selcol_d, "selcolf")
        load(arena_f[:, 0:512], mask4_d, ARF)
        A("dve", lambda e: e.tensor_copy(out=mask4b[:], in_=arena_f[:, 0:512]), ARF, ["mask4b"])
        A("dve", lambda e: e.tensor_copy(out=idb[:], in_=idf[:]), ["idf"], ["idb"])
        A("dve", lambda e: e.tensor_copy(out=selcolb[:].rearrange("p a b -> p (a b)"), in_=selcolf[:]), ["selcolf"], ["selcolb"])
        A("dve", lambda e: e.tensor_copy(out=hselb[:], in_=hsel[:]), ["hsel"], ["hselb"])
        A("dve", lambda e: e.tensor_copy(out=sel4b[:], in_=sel4[:]), ["sel4"], ["sel4b"])
        A("dve", lambda e: e.tensor_copy(out=sel16b[:], in_=sel16[:]), ["sel16"], ["sel16b"])
        A("dve", lambda e: e.tensor_copy(out=selT4b[:], in_=selT4[:]), ["selT4"], ["selT4b"])
        A("dve", lambda e: e.memset(onesb[:], 1.0), [], ["onesb"])
        A("dve", lambda e: e.memset(onesf[:], 1.0), [], ["onesf"])
        A("dve", lambda e: e.memset(v1_t[:], 1.0), [], ["v1"])

        castctr = [0]
        SFW = 3 * FW
        SBW = ABN // 2
        PW = min(SFW // 2, SBW, 2048)

        def cast_rows(src_ap, dst_ap, ncols, tag):
            i = castctr[0]
            castctr[0] += 1
            h = i % 2
            sbt = arena_b[:, h * SBW:h * SBW + ncols]
            eng = ("act", "dve")[i % 2]
            sft = arena_f[:, h * (SFW // 2):h * (SFW // 2) + ncols]
            A("sp", lambda e: e.dma_start(out=sft, in_=src_ap), writes=[("stf", h)], dma=True)
            if eng == "act":
                A("act", lambda e: e.activation(out=sbt, in_=sft, func=AF.Copy), [("stf", h)], [("stb", h)])
            else:
                A(eng, lambda e: e.tensor_copy(out=sbt, in_=sft), [("stf", h)], [("stb", h)])
            A("sp", lambda e: e.dma_start(out=dst_ap, in_=sbt), reads=[("stb", h)], writes=[tag], dma=True)

        def prepass(l):
            A("pool", lambda e: e.memset(arena_b[:, 0:2], 0.0), [], ARB + ARF + [("stb", 0), ("stb", 1), ("stf", 0), ("stf", 1)])
            wv = win_d[l].rearrange("(k p) n -> p k n", p=128)
            for k in range(8):
                for c0 in range(0, NC2, PW):
                    c1 = min(NC2, c0 + PW)
                    cast_rows(wv[:, k, c0:c1], winb_d[l][:, k, c0:c1], c1 - c0, ("winb", l))
            bv = wb_d[l].rearrange("(k p) n -> p k n", p=128)
            for k in range(16):
                cast_rows(bv[:, k, :], wbb_d[l][:, k, :], D, ("wbb", l))
            ov_ = wo_d[l].rearrange("(k p) n -> p k n", p=128)
            for k in range(8):
                cast_rows(ov_[:, k, :], wob_d[l][:, k, :], D, ("wob", l))
            A("pool", lambda e: e.memset(arena_b[:, 0:2], 0.0), [], ARB + ARF + [("stb", 0), ("stb", 1), ("stf", 0), ("stf", 1)])

        def layer_setup(l):
            load(cp[l][:], cp_d[l], ("cp", l))
            load(rp16[l][:], rp16_d[l], ("rp16", l))
            load(rp4[l][:], rp4_d[l], ("rp4", l))
            A("act", lambda e: e.activation(out=negA0[l][:], in_=rp16[l][:, 1:2], func=AF.Exp), [("rp16", l)], [("negA0", l)])
            A("dve", lambda e: e.tensor_scalar(out=negA[l][:], in0=negA0[l][:], scalar1=-1.0, scalar2=None, op0=ALU.mult),
              [("negA0", l)], [("negA", l)])
            A("dve", lambda e: e.tensor_scalar(out=nbf[l][:], in0=rp4[l][:, 1:2], scalar1=-1.0, scalar2=None, op0=ALU.mult),
              [("rp4", l)], [("nbf", l)])
            if l == 0:
                A("dve", lambda e: e.memset(lbc[l][:], 0.0), [], [("lbc", l)])
                A("dve", lambda e: e.memset(omlc[l][:], 1.0), [], [("omlc", l)])
            else:
                o0 = CP["hglb0"]
                o1 = CP["hglb1"]
                A("act", lambda e: e.activation(out=rs1[:], in_=cp[l][:, o0:o0 + 4], func=AF.Exp), [("cp", l)], ["rs1"])
                A("act", lambda e: e.activation(out=rs2[:], in_=cp[l][:, o1:o1 + 4], func=AF.Exp), [("cp", l)], ["rs2"])
                A("dve", lambda e: e.tensor_tensor(out=rs3[:], in0=rs1[:], in1=rs2[:], op=ALU.add), ["rs1", "rs2"], ["rs3"])
                A("dve", lambda e: e.reciprocal(out=rs4[:], in_=rs3[:]), ["rs3"], ["rs4"])
                A("dve", lambda e: e.tensor_tensor(out=lbc[l][:], in0=rs2[:], in1=rs4[:], op=ALU.mult), ["rs2", "rs4"], [("lbc", l)])
                A("dve", lambda e: e.tensor_tensor(out=omlc[l][:], in0=rs1[:], in1=rs4[:], op=ALU.mult), ["rs1", "rs4"], [("omlc", l)])
            A("sp", lambda e: e.dma_start(out=arena_f[:, 0:1536], in_=mlw_d[l]), writes=ARF, dma=True)
            A("dve", lambda e: e.tensor_copy(out=mlw[l][:].rearrange("p a b c -> p (a b c)"), in_=arena_f[:, 0:1536]), ARF, [("mlw", l)])
            A("sp", lambda e: e.dma_start(out=arena_f[:, 0:96], in_=wif_d[l]), writes=ARF, dma=True)
            sv = arena_f[:, 0:96].rearrange("p (h j c) -> p h j c", h=4, j=3)
            A("dve", lambda e: e.tensor_scalar(out=sv[:, :, 0, :], in0=sv[:, :, 0, :], scalar1=float(np.sqrt(128.0)), scalar2=None,
                                               op0=ALU.mult), ARF, ARF)
            A("dve", lambda e: e.tensor_copy(out=wif[l][:].rearrange("p a b -> p (a b)"), in_=arena_f[:, 0:96]), ARF, [("wif", l)])
            for t_, nm in [(haloA[l], "haloA"), (haloM[l], "haloM"), (Sssd[l], "Sssd"), (Cml[l], "Cml"),
                           (Shg[l], "Shg"), (Gprev[l], "Gprev"), (Mprev[l], "Mprev")]:
                A("pool", lambda e, t_=t_: e.memset(t_[:], 0.0), [], [(nm, l)])

        wctr = [0]

        def wload(src_ap, kc, ncols, deps):
            i = wctr[0] % NSLOT
            wctr[0] += 1
            dst = wslot[i][:, 0:kc, 0:ncols]
            A("sp", lambda e: e.dma_start(out=dst, in_=src_ap), reads=deps, writes=[("wslot", i)], dma=True)
            return wslot[i], ("wslot", i)

        evctr = [0]

        def evac_copy(out_ap, in_ap, reads, writes):
            eng = ("act", "dve")[evctr[0] % 2]
            evctr[0] += 1
            if eng == "act":
                A("act", lambda e: e.activation(out=out_ap, in_=in_ap, func=AF.Copy), reads, writes)
            else:
                A("dve", lambda e: e.tensor_copy(out=out_ap, in_=in_ap), reads, writes)

        def proj_cols(l, col0, ntiles, consume):
            t = 0
            while t < ntiles:
                n = min(6, ntiles - t)
                w, wres = wload(winb_d[l][:, :, col0 + t * 128:col0 + (t + n) * 128], 8, n * 128, [("winb", l)])
                for i in range(n):
                    ct = t + i
                    pb, pr = bank()
                    for k in range(8):
                        A("pe", lambda e, k=k, i=i, pb=pb, w=w: e.matmul(pb[:, 0:W], lhsT=w[:, k, i * 128:(i + 1) * 128], rhs=hnT[:, k, :],
                                                                         start=(k == 0), stop=(k == 7)), [wres, "hnT"], [pr])
                    consume(ct, pb, pr)
                t += n

        dctr = [0]

        def conv_tile(l, raw, ct_raw, dcol_base, ncolw, ct_idx, bias_col, out_ap, rtag, wtag):
            c = cp[l]
            di = dctr[0] % 2
            dctr[0] += 1
            for j in range(4):
                colw = dcol_base + j * ncolw + ct_idx
                if j % 2 == 0:
                    A("dve", lambda e, j=j, colw=colw: e.tensor_scalar(out=diag[:, di, j, :], in0=idf[:], scalar1=c[:, colw:colw + 1], scalar2=None, op0=ALU.mult),
                      ["idf", ("cp", l)], [("diag", di)])
                else:
                    A("act", lambda e, j=j, colw=colw: e.activation(out=diag[:, di, j, :], in_=idf[:], func=AF.Copy, scale=c[:, colw:colw + 1]),
                      ["idf", ("cp", l)], [("diag", di)])
            pb, pr = bank()
            for j in range(4):
                A("pe", lambda e, j=j, pb=pb: e.matmul(pb[:, 0:W], lhsT=diag[:, di, j, :], rhs=raw[:, ct_raw, j:j + W], start=(j == 0), stop=(j == 3)),
                  [("diag", di), rtag], [pr])
            A("act", lambda e, pb=pb: e.activation(out=out_ap, in_=pb[:, 0:W], func=AF.Silu, bias=c[:, bias_col:bias_col + 1]), [pr, ("cp", l)], [wtag])

        def rms_small(j):
            A("act", lambda e: e.activation(out=xsb[:], in_=xres[:, j, :], func=AF.Square, accum_out=ssq[:, j:j + 1]), [("xres", j)], ["xsb", "ssq"])
            A("dve", lambda e: e.tensor_scalar(out=rs1[:, j:j + 1], in0=ssq[:, j:j + 1], scalar1=1.0 / D, scalar2=EPS, op0=ALU.mult, op1=ALU.add), ["ssq"], ["rs1"])
            A("act", lambda e: e.activation(out=rs2[:, j:j + 1], in_=rs1[:, j:j + 1], func=AF.Ln), ["rs1"], ["rs2"])
            A("act", lambda e: e.activation(out=rs3[:, j:j + 1], in_=rs2[:, j:j + 1], func=AF.Exp, scale=-0.5), ["rs2"], ["rs3"])

        def layer(l, sc):
            c = cp[l]
            A("act", lambda e: e.activation(out=Sssdb[:], in_=Sssd[l][:], func=AF.Copy), [("Sssd", l)], ["Sssdb"])
            A("act", lambda e: e.activation(out=Cmlb[:], in_=Cml[l][:], func=AF.Copy), [("Cml", l)], ["Cmlb"])
            A("act", lambda e: e.activation(out=Shgb[:], in_=Shg[l][:], func=AF.Copy), [("Shg", l)], ["Shgb"])
            for j in range(NCH):
                rms_small(j)
                A("dve", lambda e, j=j: e.tensor_scalar(out=xsb[:], in0=xres[:, j, :], scalar1=rs3[:, j:j + 1], scalar2=None, op0=ALU.mult),
                  [("xres", j), "rs3"], ["xsb"])
                tb, tr = tbank()
                for k in range(8):
                    A("pe", lambda e, k=k, tb=tb: e.transpose(out=tb[:, k * 128:(k + 1) * 128], in_=xsb[:, k * 128:(k + 1) * 128], identity=idb[:]),
                      ["xsb", "idb"], [tr])
                nwv = c[:, CP["normw"]:CP["normw"] + 8].unsqueeze(2).to_broadcast([128, 8, 128])
                A("dve", lambda e, j=j, tb=tb, nwv=nwv: e.tensor_tensor(out=hnT[:, :, j * 128:(j + 1) * 128],
                                                                         in0=tb[:].rearrange("p (k t) -> p k t", k=8), in1=nwv, op=ALU.mult),
                  [tr, ("cp", l)], ["hnT"])

            rawA = view(arena_b, 0, [12, W + 4])
            convA = view(arena_b, oR1, [12, W])
            zs = view(arena_f, 0, [8, W])
            A("pool", lambda e: e.tensor_copy(out=rawA[:, :, 0:3], in_=haloA[l][:, :, 0:3]), [("haloA", l)], ["rawA"])

            def consA(ct, pb, pr):
                evac_copy(rawA[:, ct, 3:3 + W], pb[:, 0:W], [pr], ["rawA"])
            proj_cols(l, 0, 12, consA)
            A("pool", lambda e: e.tensor_copy(out=haloA[l][:, :, 0:3], in_=rawA[:, :, W:W + 3]), ["rawA"], [("haloA", l)])
            for ct in range(12):
                conv_tile(l, rawA, ct, CP["convwA"], 12, ct, CP["convbA"] + ct, convA[:, ct, :], "rawA", "convA")
            w, wr = wload(winb_d[l][:, :, 1536:1568], 8, 32, [("winb", l)])
            pb, pr = bank()
            for k in range(8):
                A("pe", lambda e, k=k, pb=pb, w=w: e.matmul(pb[0:16, 0:W], lhsT=w[:, k, 0:16], rhs=hnT[:, k, :], start=(k == 0), stop=(k == 7)), [wr, "hnT"], [pr])
            dt, la, cum, ncum, wend = r16a, r16b, r16c, r16d, r16e
            A("act", lambda e, pb=pb: e.activation(out=la[:], in_=pb[0:16, 0:W], func=AF.Exp, bias=rp16[l][:, 0:1]), [pr, ("rp16", l)], ["r16b"])
            A("act", lambda e: e.activation(out=dt[:], in_=la[:], func=AF.Ln, bias=1.0), ["r16b"], ["r16a"])
            A("dve", lambda e: e.tensor_scalar(out=wend[:], in0=dt[:], scalar1=negA[l][:], scalar2=None, op0=ALU.mult), ["r16a", ("negA", l)], ["r16e"])
            A("dve", lambda e: e.tensor_tensor_scan(out=cum[:], data0=rst128[:], data1=wend[:], initial=0.0, op0=ALU.mult, op1=ALU.add),
              ["r16e", "rst128"], ["r16c"])
            A("dve", lambda e: e.tensor_scalar(out=ncum[:], in0=cum[:], scalar1=-1.0, scalar2=None, op0=ALU.mult), ["r16c"], ["r16d"])
            A("act", lambda e: e.activation(out=ecumb[:], in_=cum[:], func=AF.Exp), ["r16c"], ["ecumb"])
            A("dve", lambda e: e.tensor_copy(out=cumhi[:], in_=cum[:]), ["r16c"], ["cumhi"])
            A("dve", lambda e: e.tensor_copy(out=cumhif[:], in_=cumhi[:]), ["cumhi"], ["cumhif"])
            A("dve", lambda e: e.tensor_tensor(out=cumlo[:], in0=cum[:], in1=cumhif[:], op=ALU.subtract), ["r16c", "cumhif"], ["cumlo"])
            A("dve", lambda e: e.tensor_scalar(out=ncumhi[:], in0=cumhi[:], scalar1=-1.0, scalar2=None, op0=ALU.mult), ["cumhi"], ["ncumhi"])
            A("dve", lambda e: e.tensor_scalar(out=ncumlo[:], in0=cumlo[:], scalar1=-1.0, scalar2=None, op0=ALU.mult), ["cumlo"], ["ncumlo"])
            for ch in range(NCH):
                A("act", lambda e, ch=ch: e.activation(out=la[:, ch * 128:(ch + 1) * 128], in_=cum[:, ch * 128:(ch + 1) * 128], func=AF.Exp,
                                                       scale=-1.0, bias=cum[:, ch * 128 + 127:ch * 128 + 128]), ["r16c"], ["r16b"])
            A("dve", lambda e: e.tensor_tensor(out=wend[:], in0=la[:], in1=dt[:], op=ALU.mult), ["r16b", "r16a"], ["r16e"])

            def consZ(ct, pb, pr):
                A("act", lambda e: e.activation(out=zs[:, ct, :], in_=pb[:, 0:W], func=AF.Silu), [pr], ["f0", "f1"])
            proj_cols(l, 1568, 8, consZ)
            for ch in range(NCH):
                t0 = ch * 128
                pb, pr = bank()
                A("pe", lambda e, pb=pb, t0=t0: e.transpose(out=pb[:, 0:16], in_=dt[:, t0:t0 + 128], identity=idf[0:16, 0:16]), ["r16a", "idf"], [pr])
                A("pe", lambda e, pb=pb, t0=t0: e.transpose(out=pb[:, 16:32], in_=wend[:, t0:t0 + 128], identity=idf[0:16, 0:16]), ["r16e", "idf"], [pr])
                A("dve", lambda e, pb=pb: e.tensor_copy(out=tok32[:], in_=pb[:, 0:32]), [pr], ["tok32"])
                A("act", lambda e, t0=t0: e.activation(out=r16g[:], in_=cum[:, t0 + 127:t0 + 128], func=AF.Exp), ["r16c"], ["r16g"])
                A("dve", lambda e: e.tensor_scalar(out=r16f[:], in0=idf[0:16, 0:16], scalar1=r16g[:], scalar2=None, op0=ALU.mult), ["r16g", "idf"], ["r16f"])
                pb2, pr2 = bank()
                A("pe", lambda e, pb2=pb2: e.matmul(pb2[:, 0:16], lhsT=onesf[0:16, 0:128], rhs=r16f[:], start=True, stop=True), ["onesf", "r16f"], [pr2])
                A("dve", lambda e, pb2=pb2: e.tensor_copy(out=decbc[:], in_=pb2[:, 0:16]), [pr2], ["decbc"])
                tb, tr = tbank()
                for ct in range(8):
                    A("pe", lambda e, ct=ct, tb=tb, t0=t0: e.transpose(out=tb[:, ct * 128:(ct + 1) * 128], in_=convA[:, ct, t0:t0 + 128], identity=idb[:]),
                      ["convA", "idb"], [tr])
                dtv = tok32[:, 0:16].unsqueeze(2).to_broadcast([128, 16, 64])
                dwv = tok32[:, 16:32].unsqueeze(2).to_broadcast([128, 16, 64])
                A("dve", lambda e, tb=tb, dtv=dtv: e.tensor_tensor(out=xdt_t[:].rearrange("p (h d) -> p h d", d=64),
                                                                    in0=tb[:].rearrange("p (h d) -> p h d", d=64), in1=dtv, op=ALU.mult), [tr, "tok32"], ["xdt"])
                A("dve", lambda e, tb=tb, dwv=dwv: e.tensor_tensor(out=xdtw_t[:].rearrange("p (h d) -> p h d", d=64),
                                                                    in0=tb[:].rearrange("p (h d) -> p h d", d=64), in1=dwv, op=ALU.mult), [tr, "tok32"], ["xdtw"])
                tb2, tr2 = tbank()
                for g in range(2):
                    A("pe", lambda e, g=g, tb2=tb2, t0=t0: e.transpose(out=tb2[:, g * 128:(g + 1) * 128], in_=convA[:, 8 + g, t0:t0 + 128], identity=idb[:]),
                      ["convA", "idb"], [tr2])
                A("act", lambda e, tb2=tb2: e.activation(out=btok_t[:], in_=tb2[:, 0:256], func=AF.Copy), [tr2], ["btok"])
                pcb, prcb = bank()
                for g in range(2):
                    A("pe", lambda e, g=g, pcb=pcb, t0=t0: e.matmul(pcb[:, g * 128:(g + 1) * 128], lhsT=convA[:, 8 + g, t0:t0 + 128],
                                                                     rhs=convA[:, 10 + g, t0:t0 + 128], start=True, stop=True), ["convA"], [prcb])
                for hq in range(4):
                    A("pool", lambda e, hq=hq, t0=t0: e.tensor_tensor(out=cdhi[:].rearrange("p (j l) -> p j l", j=4),
                                                                       in0=sel16b[:, hq, :].rearrange("p (j l) -> p j l", j=4),
                                                                       in1=cumhi[:, t0:t0 + 128].unsqueeze(1).to_broadcast([16, 4, 128]), op=ALU.mult),
                      ["sel16b", "cumhi"], ["cdhi"])
                    A("pool", lambda e, hq=hq, t0=t0: e.tensor_tensor(out=cdlo[:].rearrange("p (j l) -> p j l", j=4),
                                                                       in0=sel16b[:, hq, :].rearrange("p (j l) -> p j l", j=4),
                                                                       in1=cumlo[:, t0:t0 + 128].unsqueeze(1).to_broadcast([16, 4, 128]), op=ALU.mult),
                      ["sel16b", "cumlo"], ["cdlo"])
                    pd, prd = bank()
                    A("pe", lambda e, pd=pd, hq=hq, t0=t0: e.matmul(pd[:], lhsT=ncumhi[:, t0:t0 + 128], rhs=sel16b[:, hq, :], start=True, stop=False),
                      ["ncumhi", "sel16b"], [prd])
                    A("pe", lambda e, pd=pd, hq=hq, t0=t0: e.matmul(pd[:], lhsT=ncumlo[:, t0:t0 + 128], rhs=sel16b[:, hq, :], start=False, stop=False),
                      ["ncumlo", "sel16b"], [prd])
                    A("pe", lambda e, pd=pd: e.matmul(pd[:], lhsT=onesb[0:16, :], rhs=cdhi[:], start=False, stop=False), ["onesb", "cdhi"], [prd])
                    A("pe", lambda e, pd=pd: e.matmul(pd[:], lhsT=onesb[0:16, :], rhs=cdlo[:], start=False, stop=False), ["onesb", "cdlo"], [prd])
                    A("pe", lambda e, pd=pd: e.matmul(pd[:], lhsT=idb[:], rhs=mask4b[:], start=False, stop=True), ["idb", "mask4b"], [prd])
                    A("act", lambda e, pd=pd: e.activation(out=dec_t[:], in_=pd[:], func=AF.Exp), [prd], ["dec"])
                    g = hq // 2
                    A("dve", lambda e, hq=hq, g=g, pcb=pcb: e.tensor_tensor(out=sc_t[:, hq * 512:(hq + 1) * 512].rearrange("p (j l) -> p j l", j=4),
                                                                             in0=dec_t[:].rearrange("p (j l) -> p j l", j=4),
                                                                             in1=pcb[:, g * 128:(g + 1) * 128].unsqueeze(1).to_broadcast([128, 4, 128]),
                                                                             op=ALU.mult), ["dec", prcb], ["sc"])
                for half in range(2):
                    py1, pr1 = bank()
                    py2, pr2_ = bank()
                    pe_, pre = bank()
                    for cc in range(4):
                        ct = half * 4 + cc
                        g = ct // 4
                        for hh in range(2):
                            h = 2 * ct + hh
                            A("pe", lambda e, py1=py1, cc=cc, hh=hh, h=h: e.matmul(py1[hh * 64:(hh + 1) * 64, cc * 128:(cc + 1) * 128],
                                                                                  lhsT=xdt_t[:, h * 64:(h + 1) * 64], rhs=sc_t[:, h * 128:(h + 1) * 128],
                                                                                  start=True, stop=True), ["xdt", "sc"], [pr1])
                        A("pe", lambda e, py2=py2, cc=cc, ct=ct, g=g, t0=t0: e.matmul(py2[:, cc * 128:(cc + 1) * 128], lhsT=Sssdb[:, ct * 128:(ct + 1) * 128],
                                                                                      rhs=convA[:, 10 + g, t0:t0 + 128], start=True, stop=True), ["Sssdb", "convA"], [pr2_])
                        A("pe", lambda e, pe_=pe_, cc=cc, ct=ct, t0=t0: e.matmul(pe_[:, cc * 128:(cc + 1) * 128], lhsT=hselb[:, ct, :], rhs=ecumb[:, t0:t0 + 128],
                                                                                 start=True, stop=True), ["hselb", "ecumb"], [pre])
                    A("act", lambda e, pe_=pe_: e.activation(out=ebc_t[:], in_=pe_[:], func=AF.Copy), [pre], ["ebc"])
                    A("dve", lambda e, py2=py2: e.tensor_tensor(out=ytmp_t[:], in0=py2[:], in1=ebc_t[:], op=ALU.mult), [pr2_, "ebc"], ["ytmp"])
                    A("dve", lambda e, py1=py1: e.tensor_tensor(out=ytmp_t[:], in0=py1[:], in1=ytmp_t[:], op=ALU.add), [pr1, "ytmp"], ["ytmp"])
                    for cc in range(4):
                        ct = half * 4 + cc
                        col = CP["ssdD"] + ct
                        A("dve", lambda e, cc=cc, ct=ct, col=col, t0=t0: e.scalar_tensor_tensor(out=ytmp_t[:, cc * 128:(cc + 1) * 128], in0=convA[:, ct, t0:t0 + 128],
                                                                                                scalar=c[:, col:col + 1], in1=ytmp_t[:, cc * 128:(cc + 1) * 128],
                                                                                                op0=ALU.mult, op1=ALU.add), ["convA", "ytmp", ("cp", l)], ["ytmp"])
                    A("pool", lambda e, half=half, t0=t0: e.tensor_tensor(out=yg_t[:, half * 4:half * 4 + 4, t0:t0 + 128],
                                                                          in0=ytmp_t[:].rearrange("p (c t) -> p c t", c=4),
                                                                          in1=zs[:, half * 4:half * 4 + 4, t0:t0 + 128], op=ALU.mult), ["ytmp", "f0", "f1"], ["yg"])
                for g in range(2):
                    psl, prs = bank()
                    A("pe", lambda e, g=g, psl=psl: e.matmul(psl[:], lhsT=btok_t[:, g * 128:(g + 1) * 128], rhs=xdtw_t[:, g * 512:(g + 1) * 512],
                                                             start=True, stop=True), ["btok", "xdtw"], [prs])
                    dv = decbc[:, g * 8:(g + 1) * 8].unsqueeze(2).to_broadcast([128, 8, 64])
                    A("dve", lambda e, g=g, dv=dv: e.tensor_tensor(out=Sssd[l][:, g * 512:(g + 1) * 512].rearrange("p (h d) -> p h d", d=64),
                                                                    in0=Sssd[l][:, g * 512:(g + 1) * 512].rearrange("p (h d) -> p h d", d=64), in1=dv, op=ALU.mult),
                      [("Sssd", l), "decbc"], [("Sssd", l)])
                    A("dve", lambda e, g=g, psl=psl: e.tensor_tensor(out=Sssd[l][:, g * 512:(g + 1) * 512], in0=psl[:], in1=Sssd[l][:, g * 512:(g + 1) * 512],
                                                                      op=ALU.add), [prs, ("Sssd", l)], [("Sssd", l)])
                A("act", lambda e: e.activation(out=Sssdb[:], in_=Sssd[l][:], func=AF.Copy), [("Sssd", l)], ["Sssdb"])
            for g in range(2):
                pn, prn = bank()
                for cc in range(4):
                    ct = g * 4 + cc
                    A("act", lambda e, ct=ct, cc=cc: e.activation(out=sq_t[:, cc, :], in_=yg_t[:, ct, :], func=AF.Square), ["yg"], [("sq", cc)])
                    A("pe", lambda e, pn=pn, cc=cc: e.matmul(pn[:, 0:W], lhsT=onesb[:], rhs=sq_t[:, cc, :], start=(cc == 0), stop=(cc == 3)),
                      ["onesb", ("sq", cc)], [prn])
                A("act", lambda e, pn=pn: e.activation(out=nrm_t[:], in_=pn[:, 0:W], func=AF.Ln, scale=1.0 / 512, bias=EPS), [prn], ["nrm"])
                A("act", lambda e: e.activation(out=nrm2_t[:], in_=nrm_t[:], func=AF.Exp, scale=-0.5), ["nrm"], ["nrm2"])
                for cc in range(4):
                    ct = g * 4 + cc
                    col = CP["ssdnw"] + ct
                    A("dve", lambda e, ct=ct, col=col: e.scalar_tensor_tensor(out=yT[:, ct, :], in0=yg_t[:, ct, :], scalar=c[:, col:col + 1], in1=nrm2_t[:],
                                                                              op0=ALU.mult, op1=ALU.mult), ["yg", "nrm2", ("cp", l)], ["yg"])

            rawM = view(arena_b, 0, [4, W + 4])
            mconv = view(arena_b, 4 * (W + 4), [4, W])
            moT = view(arena_b, 4 * (W + 4) + 4 * W, [4, W])
            mzT = view(arena_b, oR1, [4, W])
            qT = view(arena_b, oR1 + 4 * W, [4, W])
            kT = view(arena_b, oR1 + 8 * W, [4, W])
            vT_t = view(arena_b, oR2, [4, W])
            A("pool", lambda e: e.tensor_copy(out=rawM[:, :, 0:3], in_=haloM[l][:, :, 0:3]), [("haloM", l)], ["rawA"])

            def consM(ct, pb, pr):
                if ct < 4:
                    evac_copy(rawM[:, ct, 3:3 + W], pb[:, 0:W], [pr], ["rawA"])
                elif ct < 8:
                    A("act", lambda e: e.activation(out=moT[:, ct - 4, :], in_=pb[:, 0:W], func=AF.Sigmoid), [pr], ["rawA"])
                else:
                    A("act", lambda e: e.activation(out=mzT[:, ct - 8, :], in_=pb[:, 0:W], func=AF.Silu), [pr], ["convA"])
            proj_cols(l, 2592, 12, consM)
            A("pool", lambda e: e.tensor_copy(out=haloM[l][:, :, 0:3], in_=rawM[:, :, W:W + 3]), ["rawA"], [("haloM", l)])
            for ct in range(4):
                conv_tile(l, rawM, ct, CP["convwM"], 4, ct, CP["convbM"] + ct, mconv[:, ct, :], "rawA", "rawA")
            qscale = float(128.0 ** -0.5)
            for h in range(4):
                pb, pr = bank()
                A("pe", lambda e, h=h, pb=pb: e.matmul(pb[:, 0:W], lhsT=mlw[l][:, 0, h, :], rhs=mconv[:, h, :], start=True, stop=True), [("mlw", l), "rawA"], [pr])
                A("act", lambda e, h=h, pb=pb: e.activation(out=qT[:, h, :], in_=pb[:, 0:W], func=AF.Copy, scale=qscale), [pr], ["convA"])
                pb, pr = bank()
                A("pe", lambda e, h=h, pb=pb: e.matmul(pb[:, 0:W], lhsT=mlw[l][:, 1, h, :], rhs=mconv[:, h, :], start=True, stop=True), [("mlw", l), "rawA"], [pr])
                A("dve", lambda e, h=h, pb=pb: e.tensor_copy(out=kT[:, h, :], in_=pb[:, 0:W]), [pr], ["convA"])
                pb, pr = bank()
                A("pe", lambda e, h=h, pb=pb: e.matmul(pb[:, 0:W], lhsT=mlw[l][:, 2, h, :], rhs=rawM[:, h, 3:3 + W], start=True, stop=True), [("mlw", l), "rawA"], [pr])
                A("act", lambda e, h=h, pb=pb: e.activation(out=vT_t[:, h, :], in_=pb[:, 0:W], func=AF.Copy), [pr], ["vT"])
            pi, pri = bank()
            pf, prf = bank()
            srcs = [qT, kT, vT_t]
            for idx in range(12):
                h, j = idx // 3, idx % 3
                A("pe", lambda e, idx=idx, h=h, j=j, pi=pi: e.matmul(pi[0:4, 0:W], lhsT=wif[l][:, idx, 0:4], rhs=srcs[j][:, h, :], start=(idx == 0), stop=(idx == 11)),
                  [("wif", l), "convA", "vT"], [pri])
            for idx in range(12):
                h, j = idx // 3, idx % 3
                A("pe", lambda e, idx=idx, h=h, j=j, pf=pf: e.matmul(pf[0:4, 0:W], lhsT=wif[l][:, idx, 4:8], rhs=srcs[j][:, h, :], start=(idx == 0), stop=(idx == 11)),
                  [("wif", l), "convA", "vT"], [prf])
            R4 = r4
            A("act", lambda e, pf=pf: e.activation(out=R4["e1"][:], in_=pf[0:4, 0:W], func=AF.Exp, scale=-1.0, bias=nbf[l][:]), [prf, ("nbf", l)], ["r4wint"])
            A("act", lambda e: e.activation(out=R4["lfn"][:], in_=R4["e1"][:], func=AF.Ln, bias=1.0), ["r4wint"], ["r4wend"])
            A("dve", lambda e: e.tensor_tensor_scan(out=R4["G"][:], data0=onesf[0:4, 0:W], data1=R4["lfn"][:], initial=Gprev[l][:],
                                                    op0=ALU.mult, op1=ALU.subtract), ["r4wend", "onesf", ("Gprev", l)], ["r4G"])
            A("dve", lambda e, pi=pi: e.scalar_tensor_tensor(out=R4["R"][:], in0=pi[0:4, 0:W], scalar=rp4[l][:, 0:1], in1=R4["G"][:], op0=ALU.add, op1=ALU.subtract),
              [pri, "r4G", ("rp4", l)], ["r4R"])
            A("dve", lambda e: e.tensor_tensor_scan(out=R4["Mt"][:], data0=onesf[0:4, 0:W], data1=R4["R"][:], initial=Mprev[l][:], op0=ALU.mult, op1=ALU.max),
              ["r4R", "onesf", ("Mprev", l)], ["r4Mt"])
            A("dve", lambda e: e.tensor_scalar(out=R4["nMt"][:], in0=R4["Mt"][:], scalar1=-1.0, scalar2=None, op0=ALU.mult), ["r4Mt"], ["r4nMt"])
            A("dve", lambda e: e.tensor_copy(out=Rhi[:], in_=R4["R"][:]), ["r4R"], ["Rhi"])
            A("dve", lambda e: e.tensor_copy(out=Rhif[:], in_=Rhi[:]), ["Rhi"], ["Rhif"])
            A("dve", lambda e: e.tensor_tensor(out=Rlo[:], in0=R4["R"][:], in1=Rhif[:], op=ALU.subtract), ["r4R", "Rhif"], ["Rlo"])
            A("dve", lambda e: e.tensor_copy(out=nMhi[:], in_=R4["nMt"][:]), ["r4nMt"], ["nMhi"])
            A("dve", lambda e: e.tensor_copy(out=nMhif[:], in_=nMhi[:]), ["nMhi"], ["nMhif"])
            A("dve", lambda e: e.tensor_tensor(out=nMlo[:], in0=R4["nMt"][:], in1=nMhif[:], op=ALU.subtract), ["r4nMt", "nMhif"], ["nMlo"])
            A("dve", lambda e: e.scalar_tensor_tensor(out=R4["t"][:], in0=R4["G"][:], scalar=-1.0, in1=R4["Mt"][:], op0=ALU.mult, op1=ALU.subtract),
              ["r4G", "r4Mt"], ["r4wint"])
            A("act", lambda e: e.activation(out=R4["emt"][:], in_=R4["t"][:], func=AF.Exp), ["r4wint"], ["r4emt"])
            for ch in range(NCH):
                t0 = ch * 128
                if ch == 0:
                    min_ap = Mprev[l][:]
                    mres = ("Mprev", l)
                else:
                    min_ap = R4["Mt"][:, t0 - 1:t0]
                    mres = "r4Mt"
                A("act", lambda e, t0=t0, min_ap=min_ap: e.activation(out=R4["wint"][:, t0:t0 + 128], in_=R4["Mt"][:, t0:t0 + 128], func=AF.Exp, scale=-1.0, bias=min_ap),
                  ["r4Mt", mres, "r4emt"], ["r4wint"])
                A("act", lambda e, t0=t0: e.activation(out=R4["wend"][:, t0:t0 + 128], in_=R4["R"][:, t0:t0 + 128], func=AF.Exp, bias=R4["nMt"][:, t0 + 127:t0 + 128]),
                  ["r4R", "r4nMt", "r4G"], ["r4wend"])
            for ch in range(NCH):
                A("dve", lambda e, ch=ch: e.tensor_scalar(out=r4s[:, ch * 4:(ch + 1) * 4], in0=idf[0:4, 0:4], scalar1=R4["wint"][:, ch * 128 + 127:ch * 128 + 128],
                                                          scalar2=None, op0=ALU.mult), ["idf", "r4wint"], ["r4s"])
            A("dve", lambda e: e.tensor_copy(out=wintb[:], in_=R4["wint"][:]), ["r4wint"], ["wintb"])
            pb, pr = bank()
            A("pe", lambda e, pb=pb: e.matmul(pb[:, 0:4 * NCH], lhsT=onesf[0:4, 0:128], rhs=r4s[:, 0:4 * NCH], start=True, stop=True), ["onesf", "r4s"], [pr])
            A("dve", lambda e, pb=pb: e.tensor_copy(out=aoldbc[:, 0:4 * NCH], in_=pb[:, 0:4 * NCH]), [pr], ["aoldbc"])
            A("pool", lambda e: e.tensor_copy(out=Gprev[l][:], in_=R4["G"][:, W - 1:W]), ["r4G"], [("Gprev", l)])
            A("pool", lambda e: e.tensor_copy(out=Mprev[l][:], in_=R4["Mt"][:, W - 1:W]), ["r4Mt", "r4wint"], [("Mprev", l)])
            hT = view(arena_f, 0, [4, W])
            for ch in range(NCH):
                t0 = ch * 128
                pb, pr = bank()
                A("pe", lambda e, pb=pb, t0=t0: e.transpose(out=pb[:, 0:4], in_=R4["wend"][:, t0:t0 + 128], identity=idf[0:4, 0:4]), ["r4wend", "idf"], [pr])
                A("dve", lambda e, pb=pb: e.tensor_copy(out=tok32[:, 0:4], in_=pb[:, 0:4]), [pr], ["tok32"])
                pk, prk = bank()
                pv, prv = bank()
                for h in range(4):
                    A("pe", lambda e, h=h, pk=pk, t0=t0: e.matmul(pk[:, h * 128:(h + 1) * 128], lhsT=mconv[:, h, t0:t0 + 128], rhs=mlw[l][:, 1, h, :], start=True, stop=True),
                      ["rawA", ("mlw", l)], [prk])
                    A("pe", lambda e, h=h, pv=pv, t0=t0: e.matmul(pv[:, h * 128:(h + 1) * 128], lhsT=rawM[:, h, 3 + t0:3 + t0 + 128], rhs=mlw[l][:, 2, h, :], start=True, stop=True),
                      ["rawA", ("mlw", l)], [prv])
                wv_ = tok32[:, 0:4].unsqueeze(2).to_broadcast([128, 4, 128])
                A("dve", lambda e, pk=pk, wv_=wv_: e.tensor_tensor(out=kw_t[:], in0=pk[:].rearrange("p (h e) -> p h e", h=4), in1=wv_, op=ALU.mult), [prk, "tok32"], ["kw"])
                A("act", lambda e, pv=pv: e.activation(out=v1_t[:, :, 0:128], in_=pv[:].rearrange("p (h e) -> p h e", h=4), func=AF.Copy), [prv], ["v1"])
                A("pool", lambda e, t0=t0: e.tensor_tensor(out=ndhi[:].rearrange("p (j l) -> p j l", j=4), in0=sel4b[:].rearrange("p (j l) -> p j l", j=4),
                                                           in1=nMhi[:, t0:t0 + 128].unsqueeze(1).to_broadcast([4, 4, 128]), op=ALU.mult), ["sel4b", "nMhi"], ["ndhi"])
                A("pool", lambda e, t0=t0: e.tensor_tensor(out=ndlo[:].rearrange("p (j l) -> p j l", j=4), in0=sel4b[:].rearrange("p (j l) -> p j l", j=4),
                                                           in1=nMlo[:, t0:t0 + 128].unsqueeze(1).to_broadcast([4, 4, 128]), op=ALU.mult), ["sel4b", "nMlo"], ["ndlo"])
                pd, prd = bank()
                A("pe", lambda e, pd=pd, t0=t0: e.matmul(pd[:], lhsT=Rhi[:, t0:t0 + 128], rhs=sel4b[:], start=True, stop=False), ["Rhi", "sel4b"], [prd])
                A("pe", lambda e, pd=pd, t0=t0: e.matmul(pd[:], lhsT=Rlo[:, t0:t0 + 128], rhs=sel4b[:], start=False, stop=False), ["Rlo", "sel4b"], [prd])
                A("pe", lambda e, pd=pd: e.matmul(pd[:], lhsT=onesb[0:4, :], rhs=ndhi[:], start=False, stop=False), ["onesb", "ndhi"], [prd])
                A("pe", lambda e, pd=pd: e.matmul(pd[:], lhsT=onesb[0:4, :], rhs=ndlo[:], start=False, stop=False), ["onesb", "ndlo"], [prd])
                A("pe", lambda e, pd=pd: e.matmul(pd[:], lhsT=idb[:], rhs=mask4b[:], start=False, stop=True), ["idb", "mask4b"], [prd])
                A("act", lambda e, pd=pd: e.activation(out=dec_t[:], in_=pd[:], func=AF.Exp), [prd], ["dec"])
                pq, prq = bank()
                for h in range(4):
                    A("pe", lambda e, h=h, pq=pq, t0=t0: e.matmul(pq[:, h * 128:(h + 1) * 128], lhsT=kT[:, h, t0:t0 + 128], rhs=qT[:, h, t0:t0 + 128], start=True, stop=True),
                      ["convA"], [prq])
                A("dve", lambda e, pq=pq: e.tensor_tensor(out=sc_t[:, 0:512], in0=pq[:], in1=dec_t[:], op=ALU.mult), [prq, "dec"], ["sc"])
                pw, prw = bank()
                for h in range(4):
                    A("pe", lambda e, h=h, pw=pw, t0=t0: e.matmul(pw[:, h * 128:(h + 1) * 128], lhsT=selT4b[:, h, :], rhs=wintb[:, t0:t0 + 128], start=True, stop=True),
                      ["selT4b", "wintb"], [prw])
                A("dve", lambda e, pw=pw, t0=t0: e.tensor_tensor(out=qw_t[:].rearrange("p (h t) -> p h t", h=4), in0=qT[:, :, t0:t0 + 128],
                                                                  in1=pw[:].rearrange("p (h t) -> p h t", h=4), op=ALU.mult), ["convA", prw], ["qw"])
                pn, prn = bank()
                for h in range(4):
                    A("pe", lambda e, h=h, pn=pn: e.matmul(pn[:, h * 128:(h + 1) * 128], lhsT=v1_t[:, h, 0:128], rhs=sc_t[:, h * 128:(h + 1) * 128], start=True, stop=False),
                      ["v1", "sc"], [prn])
                    A("pe", lambda e, h=h, pn=pn: e.matmul(pn[:, h * 128:(h + 1) * 128], lhsT=Cmlb[:, h, 0:128], rhs=qw_t[:, h * 128:(h + 1) * 128], start=False, stop=True),
                      ["Cmlb", "qw"], [prn])
                A("dve", lambda e: e.tensor_copy(out=nsel[:], in_=selcolb[:]), ["selcolb"], ["nsel"])
                for h in range(4):
                    A("dve", lambda e, h=h: e.tensor_copy(out=nsel[:, h, h:h + 1], in_=Cmlb[:, h, 128:129]), ["Cmlb", "nsel"], ["nsel"])
                pdi, prdi = bank()
                for h in range(4):
                    A("pe", lambda e, h=h, pdi=pdi: e.matmul(pdi[0:4, 0:128], lhsT=selcolb[:, h, :], rhs=sc_t[:, h * 128:(h + 1) * 128], start=(h == 0), stop=(h == 3)),
                      ["selcolb", "sc"], [prdi])
                pde, prde = bank()
                for h in range(4):
                    A("pe", lambda e, h=h, pde=pde, t0=t0: e.matmul(pde[0:4, 0:128], lhsT=nsel[:, h, :], rhs=qT[:, h, t0:t0 + 128], start=(h == 0), stop=(h == 3)),
                      ["nsel", "convA"], [prde])
                A("dve", lambda e, pde=pde, t0=t0: e.tensor_tensor(out=r4den[:], in0=pde[0:4, 0:128], in1=R4["wint"][:, t0:t0 + 128], op=ALU.mult),
                  [prde, "r4wint"], ["r4den"])
                A("dve", lambda e, pdi=pdi: e.tensor_tensor(out=r4den2[:], in0=pdi[0:4, 0:128], in1=r4den[:], op=ALU.add), [prdi, "r4den"], ["r4den2"])
                A("dve", lambda e: e.tensor_scalar(out=r4den[:], in0=r4den2[:], scalar1=-1.0, scalar2=None, op0=ALU.mult), ["r4den2"], ["r4den"])
                A("dve", lambda e: e.tensor_tensor(out=r4den2[:], in0=r4den2[:], in1=r4den[:], op=ALU.max), ["r4den2", "r4den"], ["r4den2"])
                A("dve", lambda e, t0=t0: e.tensor_tensor(out=r4den[:], in0=r4den2[:], in1=R4["emt"][:, t0:t0 + 128], op=ALU.max),
                  ["r4den2", "r4emt"], ["r4den"])
                A("dve", lambda e: e.reciprocal(out=r4rden[:], in_=r4den[:]), ["r4den"], ["r4rden"])
                A("dve", lambda e: e.tensor_copy(out=rdenb[:], in_=r4rden[:]), ["r4rden"], ["rdenb"])
                pr_, prr = bank()
                for h in range(4):
                    A("pe", lambda e, h=h, pr_=pr_: e.matmul(pr_[:, h * 128:(h + 1) * 128], lhsT=selT4b[:, h, :], rhs=rdenb[:], start=True, stop=True),
                      ["selT4b", "rdenb"], [prr])
                A("act", lambda e, pr_=pr_: e.activation(out=ebc_t[:], in_=pr_[:], func=AF.Copy), [prr], ["ebc"])
                A("dve", lambda e, pn=pn, t0=t0: e.tensor_tensor(out=hT[:, :, t0:t0 + 128], in0=pn[:].rearrange("p (h t) -> p h t", h=4),
                                                                  in1=ebc_t[:].rearrange("p (h t) -> p h t", h=4), op=ALU.mult), [prn, "ebc"], ["f0"])
                for hp in range(2):
                    pc, prc = bank()
                    for hh in range(2):
                        h = hp * 2 + hh
                        A("pe", lambda e, h=h, hh=hh, pc=pc: e.matmul(pc[:, hh * 129:(hh + 1) * 129], lhsT=kw_t[:, h, :], rhs=v1_t[:, h, :], start=True, stop=True),
                          ["kw", "v1"], [prc])
                    av = aoldbc[:, ch * 4 + hp * 2:ch * 4 + hp * 2 + 2].unsqueeze(2).to_broadcast([128, 2, 129])
                    A("dve", lambda e, hp=hp, av=av: e.tensor_tensor(out=Cml[l][:, hp * 2:hp * 2 + 2, :], in0=Cml[l][:, hp * 2:hp * 2 + 2, :], in1=av, op=ALU.mult),
                      [("Cml", l), "aoldbc"], [("Cml", l)])
                    A("dve", lambda e, hp=hp, pc=pc: e.tensor_tensor(out=Cml[l][:, hp * 2:hp * 2 + 2, :], in0=pc[:, 0:258].rearrange("p (h e) -> p h e", h=2),
                                                                      in1=Cml[l][:, hp * 2:hp * 2 + 2, :], op=ALU.add), [prc, ("Cml", l)], [("Cml", l)])
                A("act", lambda e: e.activation(out=Cmlb[:], in_=Cml[l][:], func=AF.Copy), [("Cml", l)], ["Cmlb"])
            for h in range(4):
                pm, prm = bank()
                A("pe", lambda e, h=h, pm=pm: e.matmul(pm[:, 0:W], lhsT=onesf[:, 0:128], rhs=hT[:, h, :], start=True, stop=True), ["onesf", "f0"], [prm])
                A("dve", lambda e, h=h, pm=pm: e.scalar_tensor_tensor(out=hT[:, h, :], in0=pm[:, 0:W], scalar=-1.0 / 128, in1=hT[:, h, :], op0=ALU.mult, op1=ALU.add),
                  [prm, "f0"], ["f0"])
                A("act", lambda e, h=h: e.activation(out=sqf_t[:, 0:W], in_=hT[:, h, :], func=AF.Square), ["f0"], ["ytmp"])
                pv_, prv_ = bank()
                A("pe", lambda e, pv_=pv_: e.matmul(pv_[:, 0:W], lhsT=onesf[:, 0:128], rhs=sqf_t[:, 0:W], start=True, stop=True), ["onesf", "ytmp"], [prv_])
                A("act", lambda e, pv_=pv_: e.activation(out=nrm_t[:], in_=pv_[:, 0:W], func=AF.Ln, scale=1.0 / 128, bias=EPS), [prv_], ["nrm"])
                A("act", lambda e: e.activation(out=nrm2_t[:], in_=nrm_t[:], func=AF.Exp, scale=-0.5), ["nrm"], ["nrm2"])
                cn = CP["mlnw"] + h
                cs = CP["mlskip"] + h
                A("dve", lambda e, h=h, cn=cn: e.scalar_tensor_tensor(out=hT[:, h, :], in0=hT[:, h, :], scalar=c[:, cn:cn + 1], in1=nrm2_t[:], op0=ALU.mult, op1=ALU.mult),
                  ["f0", "nrm2", ("cp", l)], ["f0"])
                A("dve", lambda e, h=h: e.tensor_tensor(out=hT[:, h, :], in0=hT[:, h, :], in1=moT[:, h, :], op=ALU.mult), ["f0", "rawA"], ["f0"])
                A("dve", lambda e, h=h, cs=cs: e.scalar_tensor_tensor(out=hT[:, h, :], in0=mconv[:, h, :], scalar=c[:, cs:cs + 1], in1=hT[:, h, :], op0=ALU.mult, op1=ALU.add),
                  ["f0", "rawA", ("cp", l)], ["f0"])
                A("pool", lambda e, h=h: e.tensor_tensor(out=yT[:, 8 + h, :], in0=hT[:, h, :], in1=mzT[:, h, :], op=ALU.mult), ["f0", "convA"], [("yT", 8 + h)])

            qs = view(arena_b, 0, [4, W])
            gzs = view(arena_b, 4 * W, [4, W])
            qtl = view(arena_b, 8 * W, [4, W])
            ktl = view(arena_b, oR1, [4, W])
            kwl = view(arena_b, oR1 + 4 * W, [4, W])
            vTh = view(arena_b, oR1 + 8 * W, [4, W])
            sg = view(arena_f, 0, [4, W])
            tmpf = view(arena_f, FW, [4, W])
            cumh = view(arena_f, 2 * FW, [4, W])
            def consH(ct, pb, pr):
                if ct < 4:
                    A("act", lambda e: e.activation(out=qs[:, ct, :], in_=pb[:, 0:W], func=AF.Silu), [pr], ["rawA"])
                elif ct < 8:
                    h = ct - 4
                    A("act", lambda e: e.activation(out=sg[:, h, :], in_=pb[:, 0:W], func=AF.Sigmoid, scale=-1.0), [pr], ["f0"])
                    A("act", lambda e: e.activation(out=cumh[:, h, :], in_=pb[:, 0:W], func=AF.Sigmoid), [pr], ["f2"])
                    A("dve", lambda e: e.scalar_tensor_tensor(out=cumh[:, h, :], in0=sg[:, h, :], scalar=lbc[l][:, h:h + 1], in1=cumh[:, h, :], op0=ALU.mult, op1=ALU.add),
                      ["f0", "f2", ("lbc", l)], ["f2"])
                else:
                    A("act", lambda e: e.activation(out=gzs[:, ct - 8, :], in_=pb[:, 0:W], func=AF.Silu), [pr], ["rawA"])
            proj_cols(l, 4128, 12, consH)
            for h in range(4):
                A("act", lambda e, h=h: e.activation(out=tmpf[:, h, :], in_=cumh[:, h, :], func=AF.Ln), ["f2"], ["f1"])
            for h in range(4):
                A("dve", lambda e, h=h: e.tensor_tensor_scan(out=cumh[:, h, :], data0=rst64[:], data1=tmpf[:, h, :], initial=0.0, op0=ALU.mult, op1=ALU.add),
                  ["f1", "rst64"], ["f2"])

            def consV(ct, pb, pr):
                evac_copy(vTh[:, ct, :], pb[:, 0:W], [pr], ["convA"])
            proj_cols(l, 5664, 4, consV)
            for h in range(4):
                A("act", lambda e, h=h: e.activation(out=tmpf[:, h, :], in_=cumh[:, h, :], func=AF.Exp), ["f2"], ["f1"])
                A("pool", lambda e, h=h: e.tensor_tensor(out=qtl[:, h, :], in0=qs[:, h, :], in1=tmpf[:, h, :], op=ALU.mult), ["rawA", "f1"], ["rawA"])
                A("dve", lambda e, h=h: e.tensor_scalar(out=nrm_t[:], in0=cumh[:, h, :], scalar1=-80.0, scalar2=-1.0, op0=ALU.max, op1=ALU.mult), ["f2"], ["nrm"])
                A("act", lambda e, h=h: e.activation(out=tmpf[:, h, :], in_=nrm_t[:], func=AF.Exp), ["nrm", "rawA"], ["f1"])
                A("dve", lambda e, h=h: e.scalar_tensor_tensor(out=ktl[:, h, :], in0=sg[:, h, :], scalar=omlc[l][:, h:h + 1], in1=tmpf[:, h, :], op0=ALU.mult, op1=ALU.mult),
                  ["f0", "f1", ("omlc", l)], ["convA"])
                for b in range(NHB):
                    A("act", lambda e, h=h, b=b: e.activation(out=tmpf[:, h, b * 64:(b + 1) * 64], in_=cumh[:, h, b * 64:(b + 1) * 64], func=AF.Exp, scale=-1.0,
                                                              bias=cumh[:, h, b * 64 + 63:b * 64 + 64]), ["f2", "convA"], ["f1"])
                A("dve", lambda e, h=h: e.scalar_tensor_tensor(out=kwl[:, h, :], in0=sg[:, h, :], scalar=omlc[l][:, h:h + 1], in1=tmpf[:, h, :], op0=ALU.mult, op1=ALU.mult),
                  ["f0", "f1", ("omlc", l)], ["convA"])
            A("act", lambda e: e.activation(out=hdec_t[:], in_=cumh[:].rearrange("p h (b t) -> p h b t", t=64)[:, :, :, 63], func=AF.Exp), ["f2"], ["hdec"])
            oT = tmpf
            for b in range(NHB):
                s0 = b * 64
                tb, tr = tbank()
                tb2, tr2 = tbank()
                for h in range(4):
                    A("pe", lambda e, h=h, tb=tb, s0=s0: e.transpose(out=tb[0:64, h * 128:(h + 1) * 128], in_=kwl[:, h, s0:s0 + 64], identity=idb[:]), ["convA", "idb"], [tr])
                    A("pe", lambda e, h=h, tb2=tb2, s0=s0: e.transpose(out=tb2[0:64, h * 128:(h + 1) * 128], in_=vTh[:, h, s0:s0 + 64], identity=idb[:]), ["convA", "idb"], [tr2])
                A("dve", lambda e, tb=tb: e.tensor_copy(out=kwtok_t[:], in_=tb[0:64, 0:512]), [tr], ["kwtok"])
                A("act", lambda e, tb2=tb2: e.activation(out=vtok_t[:], in_=tb2[0:64, 0:512], func=AF.Copy), [tr2], ["vtok"])
                pa, pra = bank()
                for h in range(4):
                    A("pe", lambda e, h=h, pa=pa, s0=s0: e.matmul(pa[0:64, h * 64:(h + 1) * 64], lhsT=ktl[:, h, s0:s0 + 64], rhs=qtl[:, h, s0:s0 + 64], start=True, stop=True),
                      ["convA", "rawA"], [pra])
                A("dve", lambda e, pa=pa: e.tensor_tensor(out=attn_t[:], in0=pa[0:64, 0:256], in1=maskhg[:], op=ALU.mult), [pra, "maskhg"], ["attn"])
                po, pro = bank()
                for h in range(4):
                    A("pe", lambda e, h=h, po=po: e.matmul(po[:, h * 64:(h + 1) * 64], lhsT=vtok_t[:, h * 128:(h + 1) * 128], rhs=attn_t[:, h * 64:(h + 1) * 64], start=True, stop=False),
                      ["vtok", "attn"], [pro])
                    A("pe", lambda e, h=h, po=po, s0=s0: e.matmul(po[:, h * 64:(h + 1) * 64], lhsT=Shgb[:, h, :], rhs=qtl[:, h, s0:s0 + 64], start=False, stop=True),
                      ["Shgb", "rawA"], [pro])
                A("act", lambda e, po=po, s0=s0: e.activation(out=oT[:, :, s0:s0 + 64], in_=po[:, 0:256].rearrange("p (h t) -> p h t", h=4), func=AF.Copy), [pro], ["f1"])
                pS, prS = bank()
                for h in range(4):
                    A("pe", lambda e, h=h, pS=pS: e.matmul(pS[:, h * 128:(h + 1) * 128], lhsT=kwtok_t[:, h * 128:(h + 1) * 128], rhs=vtok_t[:, h * 128:(h + 1) * 128], start=True, stop=True),
                      ["kwtok", "vtok"], [prS])
                dv = hdec_t[:, :, b:b + 1].to_broadcast([128, 4, 128])
                A("dve", lambda e, dv=dv: e.tensor_tensor(out=Shg[l][:], in0=Shg[l][:], in1=dv, op=ALU.mult), [("Shg", l), "hdec"], [("Shg", l)])
                A("dve", lambda e, pS=pS: e.tensor_tensor(out=Shg[l][:], in0=pS[:].rearrange("p (h v) -> p h v", h=4), in1=Shg[l][:], op=ALU.add), [prS, ("Shg", l)], [("Shg", l)])
                A("act", lambda e: e.activation(out=Shgb[:], in_=Shg[l][:], func=AF.Copy), [("Shg", l)], ["Shgb"])
            for h in range(4):
                A("act", lambda e, h=h: e.activation(out=sq_t[:, h, :], in_=oT[:, h, :], func=AF.Square), ["f1"], [("sq", h)])
                pn, prn = bank()
                A("pe", lambda e, h=h, pn=pn: e.matmul(pn[:, 0:W], lhsT=onesb[:], rhs=sq_t[:, h, :], start=True, stop=True), ["onesb", ("sq", h)], [prn])
                A("act", lambda e, pn=pn: e.activation(out=nrm_t[:], in_=pn[:, 0:W], func=AF.Ln, scale=1.0 / 128, bias=EPS), [prn], ["nrm"])
                A("act", lambda e: e.activation(out=nrm2_t[:], in_=nrm_t[:], func=AF.Exp, scale=-0.5), ["nrm"], ["nrm2"])
                cn = CP["hgnw"] + h
                A("dve", lambda e, h=h, cn=cn: e.scalar_tensor_tensor(out=oT[:, h, :], in0=oT[:, h, :], scalar=c[:, cn:cn + 1], in1=nrm2_t[:], op0=ALU.mult, op1=ALU.mult),
                  ["f1", "nrm2", ("cp", l)], ["f1"])
                A("pool", lambda e, h=h: e.tensor_tensor(out=yT[:, 12 + h, :], in0=oT[:, h, :], in1=gzs[:, h, :], op=ALU.mult), ["f1", "rawA"], [("yT", 12 + h)])

            gat = view(arena_b, 0, [24, W])
            def consG(ct, pb, pr):
                A("act", lambda e: e.activation(out=gat[:, ct, :], in_=pb[:, 0:W], func=AF.Sigmoid), [pr], ["rawA", "convA"])
            proj_cols(l, 6176, 24, consG)
            mt = view(arena_f, 0, [2, W])
            GR = ["rawA", "convA"]
            for cq in range(8):
                if cq % 4 == 0:
                    hb = cq // 4
                    w1, wr1 = wload(wbb_d[l][:, 0:8, hb * 512:(hb + 1) * 512], 8, 512, [("wbb", l)])
                    w2, wr2 = wload(wbb_d[l][:, 8:16, hb * 512:(hb + 1) * 512], 8, 512, [("wbb", l)])
                cq4 = cq % 4
                p0, pr0 = bank()
                p1, pr1 = bank()
                p2, pr2 = bank()
                for k in range(8):
                    A("pe", lambda e, k=k, cq4=cq4, p0=p0, w1=w1: e.matmul(p0[:, 0:W], lhsT=w1[:, k, cq4 * 128:(cq4 + 1) * 128], rhs=yT[:, k, :], start=(k == 0), stop=(k == 7)),
                      [wr1, "yg"], [pr0])
                for k in range(4):
                    A("pe", lambda e, k=k, cq4=cq4, p1=p1, w2=w2: e.matmul(p1[:, 0:W], lhsT=w2[:, k, cq4 * 128:(cq4 + 1) * 128], rhs=yT[:, 8 + k, :], start=(k == 0), stop=(k == 3)),
                      [wr2, ("yT", 8 + k)], [pr1])
                for k in range(4):
                    A("pe", lambda e, k=k, cq4=cq4, p2=p2, w2=w2: e.matmul(p2[:, 0:W], lhsT=w2[:, 4 + k, cq4 * 128:(cq4 + 1) * 128], rhs=yT[:, 12 + k, :], start=(k == 0), stop=(k == 3)),
                      [wr2, ("yT", 12 + k)], [pr2])
                A("dve", lambda e, cq=cq, p0=p0: e.tensor_tensor(out=mt[:, 0, :], in0=p0[:, 0:W], in1=gat[:, cq, :], op=ALU.mult), [pr0] + GR, ["f0"])
                A("dve", lambda e, cq=cq, p1=p1: e.tensor_tensor(out=mt[:, 1, :], in0=p1[:, 0:W], in1=gat[:, 8 + cq, :], op=ALU.mult), [pr1, "f0"] + GR, ["f0b"])
                A("pool", lambda e: e.tensor_tensor(out=mt[:, 0, :], in0=mt[:, 0, :], in1=mt[:, 1, :], op=ALU.add), ["f0", "f0b"], ["f0"])
                A("dve", lambda e, cq=cq, p2=p2: e.tensor_tensor(out=mt[:, 1, :], in0=p2[:, 0:W], in1=gat[:, 16 + cq, :], op=ALU.mult), [pr2, "f0"] + GR, ["f0b"])
                A("pool", lambda e, cq=cq: e.tensor_tensor(out=mergedT[:, cq, :], in0=mt[:, 0, :], in1=mt[:, 1, :], op=ALU.add), ["f0", "f0b"], ["hnT"])
            for n in range(2):
                w, wr = wload(wob_d[l][:, :, n * 512:(n + 1) * 512], 8, 512, [("wob", l)])
                for j in range(NCH):
                    pb, pr = bank()
                    for k in range(8):
                        A("pe", lambda e, k=k, j=j, pb=pb, w=w: e.matmul(pb[:], lhsT=mergedT[:, k, j * 128:(j + 1) * 128], rhs=w[:, k, 0:512],
                                                                         start=(k == 0), stop=(k == 7)), ["hnT", wr], [pr])
                    A("dve", lambda e, j=j, n=n, pb=pb: e.tensor_tensor(out=xres[:, j, n * 512:(n + 1) * 512], in0=pb[:], in1=xres[:, j, n * 512:(n + 1) * 512], op=ALU.add),
                      [pr, ("xres", j)], [("xres", j)])

        for l in range(depth):
            layer_setup(l)
        prepass(0)
        xv = x_d.rearrange("(s j p) d -> s p j d", p=128, j=NCH)
        ov = out_d.rearrange("(s j p) d -> s p j d", p=128, j=NCH)
        fnw_v = arena_f[:, 2 * FW:2 * FW + D]
        outs = []
        for sc in range(nsc):
            for j in range(NCH):
                A("act", lambda e, sc=sc, j=j: e.dma_start(out=xres[:, j, :], in_=xv[sc, :, j, :]), writes=[("xres", j)], dma=True)
            for l in range(depth):
                if sc == 0 and l >= 1:
                    prepass(l)
                layer(l, sc)
            A("act", lambda e: e.dma_start(out=fnw_v, in_=fnw_d), writes=["f2"], dma=True)
            for j in range(NCH):
                rms_small(j)
                A("dve", lambda e, j=j: e.scalar_tensor_tensor(out=xres[:, j, :], in0=xres[:, j, :], scalar=rs3[:, j:j + 1], in1=fnw_v, op0=ALU.mult, op1=ALU.mult),
                  [("xres", j), "rs3", "f2"], [("xres", j)])
                A("act", lambda e, sc=sc, j=j: e.dma_start(out=ov[sc, :, j, :], in_=xres[:, j, :]), reads=[("xres", j)], writes=[("out", sc, j)], dma=True)
                outs.append(("out", sc, j))
        A("sp", lambda e: e.nop(), reads=outs)

        sems = {e: es.enter_context(nc.semaphore("sem_" + e)) for e in S.ENGS}
        dsems = [es.enter_context(nc.semaphore(f"dsem{i}")) for i in range(S.n_dma_sems)]
        S.finalize()
        print("ops:", len(S.ops))
        S.emit(nc, sems, dsems)
    return nc


def host_consts():
    cst = {}
    cst["ident"] = np.eye(128, dtype=np.float32)
    s = np.arange(128)[:, None]
    lq = np.arange(128)[None, :]
    m = np.where(lq >= s, 0.0, NEG).astype(np.float32)
    cst["mask4"] = np.tile(m, (1, 4))
    s6 = np.arange(64)[:, None]
    l6 = np.arange(64)[None, :]
    cst["maskhg"] = np.tile((l6 >= s6).astype(np.float32), (1, 4))
    sel16 = np.zeros((16, 4, 4, 128), np.float32)
    for hq in range(4):
        for j in range(4):
            sel16[4 * hq + j, hq, j, :] = 1.0
    cst["sel16"] = sel16.reshape(16, 2048)
    sel4 = np.zeros((4, 4, 128), np.float32)
    for h in range(4):
        sel4[h, h, :] = 1.0
    cst["sel4"] = sel4.reshape(4, 512)
    hsel = np.zeros((16, 8, 128), np.float32)
    for ct in range(8):
        for m_ in range(128):
            hsel[2 * ct + m_ // 64, ct, m_] = 1.0
    cst["hsel"] = hsel.reshape(16, 1024)
    selT4 = np.zeros((4, 4, 128), np.float32)
    for h in range(4):
        selT4[h, h, :] = 1.0
    cst["selT4"] = selT4.reshape(4, 512)
    r = np.ones((16, 512), np.float32)
    r[:, 0::128] = 0.0
    cst["rst128"] = r
    r = np.ones((128, 512), np.float32)
    r[:, 0::HC] = 0.0
    cst["rst64"] = r
    selcol = np.zeros((128, 4, 4), np.float32)
    for h in range(4):
        selcol[:, h, h] = 1.0
    cst["selcol"] = selcol.reshape(128, 16)
    return cst


def col(v, n):
    return np.ascontiguousarray(np.asarray(v, np.float32).reshape(n, 128).T)


def host_layer_inputs(l, I):
    f = np.float32
    w_in = np.asarray(I["w_in"][l], f)
    o = {}
    win = np.zeros((D, NC2), f)
    src = [(0, 1536, cA), (1552, 2576, cZ), (1536, 1552, cDT), (2576, 3088, cMX), (3088, 3600, cMO), (3600, 4112, cMZ),
           (4112, 4624, cGQ), (4624, 5136, cGF), (5648, 6160, cGZ), (5136, 5648, cGI), (6160, 9232, cGATE)]
    for a, b, d in src:
        win[:, d:d + (b - a)] = w_in[:, a:b]
    o[f"win{l}"] = win
    o[f"wb{l}"] = np.ascontiguousarray(np.concatenate([I["w_branch_ssd"][l], I["w_branch_ml"][l], I["w_branch_hg"][l]], 0).astype(f))
    o[f"wo{l}"] = np.ascontiguousarray(np.asarray(I["w_out"][l], f))
    mlw = np.stack([I["ml_wq"][l], I["ml_wk"][l], I["ml_wv"][l]], 0).astype(f)
    o[f"mlw{l}"] = np.ascontiguousarray(mlw.transpose(2, 0, 1, 3).reshape(128, 1536))
    wif = np.asarray(I["ml_w_if"][l], f).reshape(4, 3, 128, 8)
    o[f"wif{l}"] = np.ascontiguousarray(wif.transpose(2, 0, 1, 3).reshape(128, 96))
    cpm = np.zeros((128, NCP), f)
    cpm[:, CP["normw"]:CP["normw"] + 8] = col(I["norm_w"][l], 8)
    cpm[:, CP["convbA"]:CP["convbA"] + 12] = col(I["ssd_conv_b"][l], 12)
    for j in range(4):
        cpm[:, CP["convwA"] + j * 12:CP["convwA"] + (j + 1) * 12] = col(I["ssd_conv_w"][l][j], 12)
        cpm[:, CP["convwM"] + j * 4:CP["convwM"] + (j + 1) * 4] = col(I["ml_conv_w"][l][j], 4)
    cpm[:, CP["convbM"]:CP["convbM"] + 4] = col(I["ml_conv_b"][l], 4)
    cpm[:, CP["ssdD"]:CP["ssdD"] + 8] = col(np.repeat(np.asarray(I["ssd_d"][l], f), 64), 8)
    cpm[:, CP["ssdnw"]:CP["ssdnw"] + 8] = col(I["ssd_norm_w"][l], 8)
    cpm[:, CP["mlnw"]:CP["mlnw"] + 4] = col(I["ml_norm_w"][l], 4)
    cpm[:, CP["mlskip"]:CP["mlskip"] + 4] = col(I["ml_skip"][l], 4)
    cpm[:, CP["hgnw"]:CP["hgnw"] + 4] = col(I["hg_norm_w"][l], 4)
    cpm[:, CP["hglb0"]:CP["hglb0"] + 4] = col(I["hg_lower_bounds"][0], 4)
    cpm[:, CP["hglb1"]:CP["hglb1"] + 4] = col(I["hg_lower_bounds"][1], 4)
    o[f"cp{l}"] = cpm
    o[f"rp16_{l}"] = np.ascontiguousarray(np.stack([I["ssd_dt_bias"][l], I["ssd_a_log"][l]], 1).astype(f))
    bif = np.asarray(I["ml_b_if"][l], f)
    o[f"rp4_{l}"] = np.ascontiguousarray(np.stack([bif[0:4], bif[4:8]], 1))
    return o


_CACHE = {}


def kernel(**inputs):
    I = {k: np.asarray(v) for k, v in inputs.items()}
    x = np.asarray(I["x"], np.float32)
    shared = dict(host_consts())
    for l in range(DEPTH):
        shared.update(host_layer_inputs(l, I))
    shared["fnw"] = np.ascontiguousarray(np.broadcast_to(np.asarray(I["final_norm_w"], np.float32)[None, :], (128, D)))
    if "nc" not in _CACHE:
        _CACHE["nc"] = build_program()
    nc = _CACHE["nc"]
    in_maps = []
    for c in range(8):
        m = dict(shared)
        m["x"] = np.ascontiguousarray(x[c % 4])
        in_maps.append(m)
    res = run_bass_kernel_spmd(nc, in_maps, core_ids=list(range(8)))
    out = np.stack([np.asarray(res.results[b]["out"], np.float32) for b in range(4)], 0)
    return out
```

```python
import numpy as np
from contextlib import ExitStack
import concourse.bass as bass
import concourse.mybir as mybir
from concourse.bass_utils import run_bass_kernel_spmd
from concourse.alu_op_type import AluOpType as ALU

AF = mybir.ActivationFunctionType
F32 = mybir.dt.float32
BF16 = mybir.dt.bfloat16

D = 1024
T = 4096
DEPTH = 2
SC = 256
NC2 = 9248
cA, cZ, cDT, cMX, cMO, cMZ, cGQ, cGF, cGZ, cGI, cGATE = 0, 1568, 1536, 2592, 3104, 3616, 4128, 4640, 5152, 5664, 6176
WGROUPS = [(0, 1536), (1536, 2592), (2592, 4128), (4128, 5664), (5664, 6176), (6176, 7712), (7712, 9248)]
HC = 64
EPS = 1e-6
NEG = -30000.0


class _Op:
    __slots__ = ("eng", "fn", "deps", "dma", "waits", "sig", "sigval", "dsem", "dval", "dprev")

    def __init__(self, eng, fn, deps, dma):
        self.eng = eng
        self.fn = fn
        self.deps = deps
        self.dma = dma
        self.waits = []
        self.sig = False
        self.sigval = 0
        self.dsem = -1
        self.dval = 0
        self.dprev = 0


class Sched:
    ENGS = ("pe", "act", "dve", "pool", "sp")

    def __init__(self, n_dma_sems=24, same_sync=True):
        self.ops = []
        self.res_w = {}
        self.res_r = {}
        self.n_dma_sems = n_dma_sems
        self.same_sync = same_sync

    def add(self, eng, fn, reads=(), writes=(), dma=False):
        deps = {}
        for r in reads:
            w = self.res_w.get(r)
            if w is not None:
                deps[w] = 2
        for r in writes:
            w = self.res_w.get(r)
            if w is not None:
                deps[w] = max(deps.get(w, 0), 1)
            rr = self.res_r.get(r)
            if rr:
                for v in rr.values():
                    deps.setdefault(v, 0)
        i = len(self.ops)
        self.ops.append(_Op(eng, fn, deps, dma))
        key = ("dma", i) if dma else eng
        for r in reads:
            self.res_r.setdefault(r, {})[key] = i
        for r in writes:
            self.res_w[r] = i
            self.res_r[r] = {}
        return i

    def finalize(self):
        ops = self.ops
        know = {e: {} for e in self.ENGS}
        know_dma = {e: {} for e in self.ENGS}
        clock = {}
        needed = set()
        for op in ops:
            needed.update(op.deps)
        dma_uses = [0] * self.n_dma_sems
        dma_rr = 0
        for i, op in enumerate(ops):
            E = op.eng
            kn = know[E]
            waits = {}
            dwaits = {}
            for d in op.deps:
                dop = ops[d]
                if dop.dma:
                    if know_dma[E].get(dop.dsem, 0) >= dop.dval:
                        continue
                    dwaits[dop.dsem] = max(dwaits.get(dop.dsem, 0), dop.dval)
                    ck = clock.get(d)
                    if ck:
                        for k, v in ck.items():
                            if kn.get(k, -1) < v:
                                kn[k] = v
                else:
                    E2 = dop.eng
                    if E2 == E and (E == "pe" or E == "sp" or not self.same_sync):
                        continue
                    if E2 == E and self.same_sync == "raw" and op.deps[d] < 2:
                        continue
                    if E2 == E and self.same_sync == "rawwaw" and op.deps[d] < 1:
                        continue
                    if kn.get(E2, -1) >= d:
                        continue
                    waits[E2] = max(waits.get(E2, -1), d)
            for E2, d in waits.items():
                if kn.get(E2, -1) >= d:
                    continue
                ck = clock[d]
                for k, v in ck.items():
                    if kn.get(k, -1) < v:
                        kn[k] = v
                ops[d].sig = True
                op.waits.append(("c", E2, d))
            for s, v in dwaits.items():
                know_dma[E][s] = v
                op.waits.append(("d", s, v))
            if op.dma:
                s = dma_rr
                dma_rr = (dma_rr + 1) % self.n_dma_sems
                op.dprev = dma_uses[s] * 16
                dma_uses[s] += 1
                op.dsem = s
                op.dval = dma_uses[s] * 16
                if op.dprev > 0 and know_dma[E].get(s, 0) < op.dprev:
                    op.waits.append(("d", s, op.dprev))
                    know_dma[E][s] = op.dprev
            if i in needed:
                ck = dict(kn)
                if not op.dma:
                    ck[E] = i
                clock[i] = ck
        cnt = {e: 0 for e in self.ENGS}
        for op in ops:
            if op.sig and not op.dma:
                cnt[op.eng] += 1
                op.sigval = cnt[op.eng]

    def emit(self, nc, sems, dsems):
        ops = self.ops
        per = {e: [] for e in self.ENGS}
        for op in ops:
            per[op.eng].append(op)

        def replay(name, eng):
            for op in per[name]:
                for w in op.waits:
                    if w[0] == "c":
                        eng.wait_ge(sems[w[1]], ops[w[2]].sigval)
                    else:
                        eng.wait_ge(dsems[w[1]], w[2])
                ins = op.fn(eng)
                if op.dma:
                    ins.then_inc(dsems[op.dsem], 16)
                elif op.sig:
                    ins.then_inc(sems[op.eng], 1)

        with nc.Block() as block:
            @block.tensor
            def _(e):
                replay("pe", e)

            @block.scalar
            def _(e):
                replay("act", e)

            @block.vector
            def _(e):
                replay("dve", e)

            @block.gpsimd
            def _(e):
                replay("pool", e)

            @block.sync
            def _(e):
                replay("sp", e)


CP = {}
_o = 0
for _n, _w in [("normw", 8), ("convbA", 12), ("convwA", 48), ("convbM", 4), ("convwM", 16), ("ssdD", 8),
               ("ssdnw", 8), ("mlnw", 4), ("mlskip", 4), ("hgnw", 4), ("hglb0", 4), ("hglb1", 4)]:
    CP[_n] = _o
    _o += _w
NCP = _o


def build_program(nsc=None, depth=DEPTH, same_sync="rawwaw"):
    W = SC
    NCH = SC // 128
    NHB = SC // HC
    if nsc is None:
        nsc = T // SC
    nc = bass.Bass("TRN2", target_bir_lowering=False)
    S = Sched(same_sync=same_sync)
    A = S.add

    def din(name, shape, dt=F32):
        return nc.dram_tensor(name, list(shape), dt, kind="ExternalInput").ap()

    x_d = din("x", [T, D])
    out_d = nc.dram_tensor("out", [T, D], F32, kind="ExternalOutput").ap()
    win_d = [din(f"win{l}", [D, NC2]) for l in range(DEPTH)]
    wb_d = [din(f"wb{l}", [2048, D]) for l in range(DEPTH)]
    wo_d = [din(f"wo{l}", [D, D]) for l in range(DEPTH)]
    mlw_d = [din(f"mlw{l}", [128, 3 * 4 * 128]) for l in range(DEPTH)]
    wif_d = [din(f"wif{l}", [128, 96]) for l in range(DEPTH)]
    cp_d = [din(f"cp{l}", [128, NCP]) for l in range(DEPTH)]
    rp16_d = [din(f"rp16_{l}", [16, 2]) for l in range(DEPTH)]
    rp4_d = [din(f"rp4_{l}", [4, 2]) for l in range(DEPTH)]
    fnw_d = din("fnw", [128, D])
    ident_d = din("ident", [128, 128])
    mask4_d = din("mask4", [128, 512])
    maskhg_d = din("maskhg", [64, 256])
    sel16_d = din("sel16", [16, 4 * 512])
    sel4_d = din("sel4", [4, 512])
    hsel_d = din("hsel", [16, 8 * 128])
    selT4_d = din("selT4", [4, 4 * 128])
    rst128_d = din("rst128", [16, 512])
    rst64_d = din("rst64", [128, 512])
    selcol_d = din("selcol", [128, 16])
    winb_d = [nc.dram_tensor(f"winb{l}", [128, 8, NC2], BF16, kind="Internal").ap() for l in range(DEPTH)]
    wbb_d = [nc.dram_tensor(f"wbb{l}", [128, 16, D], BF16, kind="Internal").ap() for l in range(DEPTH)]
    wob_d = [nc.dram_tensor(f"wob{l}", [128, 8, D], BF16, kind="Internal").ap() for l in range(DEPTH)]

    with ExitStack() as es:
        tot = [0]

        def sb(name, shape, dt):
            n = 1
            for v in shape[1:]:
                n *= v
            tot[0] += n * (2 if dt == BF16 else 4)
            return es.enter_context(nc.sbuf_tensor("s_" + name, list(shape), dt))

        def pst(name, shape, dt):
            return es.enter_context(nc.psum_tensor(name, list(shape), dt))

        NB = 6
        banks = [pst(f"pb{i}", [128, 512], F32) for i in range(NB)]
        tbanks = [pst(f"tb{i}", [128, 1024], BF16) for i in range(2)]
        bctr = [0]
        tctr = [0]

        def bank():
            i = bctr[0] % NB
            bctr[0] += 1
            return banks[i], ("pb", i)

        def tbank():
            i = tctr[0] % 2
            tctr[0] += 1
            return tbanks[i], ("tb", i)

        idf = sb("idf", [128, 128], F32)
        idb = sb("idb", [128, 128], BF16)
        onesb = sb("onesb", [128, 128], BF16)
        onesf = sb("onesf", [128, 512], F32)
        mask4b = sb("mask4b", [128, 512], BF16)
        maskhg = sb("maskhg", [64, 256], F32)
        sel16 = sb("sel16", [16, 4, 512], F32)
        sel4 = sb("sel4", [4, 512], F32)
        hsel = sb("hsel", [16, 8, 128], F32)
        selT4 = sb("selT4", [4, 4, 128], F32)
        rst128 = sb("rst128", [16, W], F32)
        rst64 = sb("rst64", [128, W], F32)
        selcolf = sb("selcolf", [128, 16], F32)
        selcolb = sb("selcolb", [128, 4, 4], BF16)

        cp = [sb(f"cp{l}", [128, NCP], F32) for l in range(depth)]
        rp16 = [sb(f"rp16_{l}", [16, 2], F32) for l in range(depth)]
        rp4 = [sb(f"rp4_{l}", [4, 2], F32) for l in range(depth)]
        negA = [sb(f"negA{l}", [16, 1], F32) for l in range(depth)]
        negA0 = [sb(f"negA0{l}", [16, 1], F32) for l in range(depth)]
        nbf = [sb(f"nbf{l}", [4, 1], F32) for l in range(depth)]
        lbc = [sb(f"lbc{l}", [128, 4], F32) for l in range(depth)]
        omlc = [sb(f"omlc{l}", [128, 4], F32) for l in range(depth)]
        mlw = [sb(f"mlw{l}", [128, 3, 4, 128], BF16) for l in range(depth)]
        wif = [sb(f"wif{l}", [128, 12, 8], BF16) for l in range(depth)]
        haloA = [sb(f"haloA{l}", [128, 12, 4], BF16) for l in range(depth)]
        haloM = [sb(f"haloM{l}", [128, 4, 4], BF16) for l in range(depth)]
        Sssd = [sb(f"Sssd{l}", [128, 1024], F32) for l in range(depth)]
        Cml = [sb(f"Cml{l}", [128, 4, 129], F32) for l in range(depth)]
        Shg = [sb(f"Shg{l}", [128, 4, 128], F32) for l in range(depth)]
        Gprev = [sb(f"Gprev{l}", [4, 1], F32) for l in range(depth)]
        Mprev = [sb(f"Mprev{l}", [4, 1], F32) for l in range(depth)]
        Sssdb = sb("Sssdb", [128, 1024], BF16)
        Cmlb = sb("Cmlb", [128, 4, 129], BF16)
        Shgb = sb("Shgb", [128, 4, 128], BF16)
        diag = sb("diag", [128, 2, 4, 128], BF16)

        NSLOT = 4
        SLW = 768
        wslot = [sb(f"wslot{i}", [128, 8, SLW], BF16) for i in range(NSLOT)]
        xres = sb("xres", [128, NCH, D], F32)
        hnT = sb("hnT", [128, 8, W], BF16)
        oR1 = 12 * (W + 4)
        oR2 = oR1 + 12 * W
        ABN = oR2 + 4 * W
        arena_b = sb("arena_b", [128, ABN], BF16)
        FW = 4 * W
        arena_f = sb("arena_f", [128, 3 * FW], F32)
        yT = sb("yT", [128, 16, W], BF16)
        mergedT = hnT
        ssq = sb("ssq", [128, 4], F32)
        rs1 = sb("rs1", [128, 4], F32)
        rs2 = sb("rs2", [128, 4], F32)
        rs3 = sb("rs3", [128, 4], F32)
        rs4 = sb("rs4", [128, 4], F32)
        xsb = sb("xsb", [128, D], BF16)
        tok32 = sb("tok32", [128, 32], F32)
        decbc = sb("decbc", [128, 16], F32)
        r16a = sb("r16a", [16, W], F32)
        r16b = sb("r16b", [16, W], F32)
        r16c = sb("r16c", [16, W], F32)
        r16d = sb("r16d", [16, W], F32)
        r16e = sb("r16e", [16, W], F32)
        r16f = sb("r16f", [16, 16], F32)
        r16g = sb("r16g", [16, 1], F32)
        cumdiag = sb("cumdiag", [16, 512], F32)
        r4 = {n: sb("r4" + n, [4, W], F32) for n in ["G", "R", "Mt", "nMt", "emt", "wint", "wend"]}
        r4["e1"] = r4["wint"]
        r4["lfn"] = r4["wend"]
        r4["t"] = r4["wint"]
        r4den = sb("r4den", [4, 128], F32)
        r4den2 = sb("r4den2", [4, 128], F32)
        r4rden = sb("r4rden", [4, 128], F32)
        r4s = sb("r4s", [4, 16], F32)
        nmdiag = sb("nmdiag", [4, 512], F32)
        aoldbc = sb("aoldbc", [128, 16], F32)
        nsel = sb("nsel", [128, 4, 4], BF16)
        xdt_t = sb("xdt_t", [128, 1024], BF16)
        xdtw_t = sb("xdtw_t", [128, 1024], BF16)
        btok_t = sb("btok_t", [128, 256], BF16)
        dec_t = sb("dec_t", [128, 512], BF16)
        sc_t = sb("sc_t", [128, 2048], BF16)
        ebc_t = sb("ebc_t", [128, 512], F32)
        ytmp_t = sb("ytmp_t", [128, 512], F32)
        sq_t = sb("sq_t", [128, 4, W], BF16)
        nrm_t = sb("nrm_t", [128, W], F32)
        nrm2_t = sb("nrm2_t", [128, W], F32)
        kw_t = sb("kw_t", [128, 4, 128], BF16)
        v1_t = sb("v1_t", [128, 4, 129], BF16)
        qw_t = sb("qw_t", [128, 512], BF16)
        hdec_t = sb("hdec_t", [128, 4, NHB], F32)
        kwtok_t = sb("kwtok_t", [64, 512], BF16)
        vtok_t = sb("vtok_t", [64, 512], BF16)
        attn_t = sb("attn_t", [64, 256], BF16)
        yg_t = yT
        sel16b = sb("sel16b", [16, 4, 512], BF16)
        cumhi = sb("cumhi", [16, W], BF16)
        cumhif = sb("cumhif", [16, W], F32)
        cumlo = sb("cumlo", [16, W], BF16)
        ncumhi = sb("ncumhi", [16, W], BF16)
        ncumlo = sb("ncumlo", [16, W], BF16)
        cdhi = sb("cdhi", [16, 512], BF16)
        cdlo = sb("cdlo", [16, 512], BF16)
        hselb = sb("hselb", [16, 8, 128], BF16)
        selT4b = sb("selT4b", [4, 4, 128], BF16)
        ecumb = sb("ecumb", [16, W], BF16)
        wintb = sb("wintb", [4, W], BF16)
        rdenb = sb("rdenb", [4, 128], BF16)
        sqf_t = ytmp_t
        print("SBUF bytes/partition:", tot[0])

        def view(base, off, shape):
            n = 1
            for v in shape:
                n *= v
            v = base[:, off:off + n]
            if len(shape) == 2:
                return v.rearrange("p (a b) -> p a b", a=shape[0])
            return v

        ARB = ["rawA", "convA", "vT"]
        ARF = ["f0", "f1", "f2"]

        def load(dst, src, res):
            A("sp", lambda e: e.dma_start(out=dst, in_=src), writes=[res] if not isinstance(res, list) else res, dma=True)

        load(idf[:], ident_d, "idf")
        load(maskhg[:], maskhg_d, "maskhg")
        load(sel16[:], sel16_d.rearrange("p (a b) -> p a b", a=4), "sel16")
        load(sel4[:], sel4_d, "sel4")
        load(hsel[:], hsel_d.rearrange("p (a b) -> p a b", a=8), "hsel")
        load(selT4[:], selT4_d.rearrange("p (a b) -> p a b", a=4), "selT4")
        load(rst128[:], rst128_d[:, 0:W], "rst128")
        load(rst64[:], rst64_d[:, 0:W], "rst64")
        load(selcolf[:], selcol_d, "selcolf")
        load(arena_f[:, 0:512], mask4_d, ARF)
        A("dve", lambda e: e.tensor_copy(out=mask4b[:], in_=arena_f[:, 0:512]), ARF, ["mask4b"])
        A("dve", lambda e: e.tensor_copy(out=idb[:], in_=idf[:]), ["idf"], ["idb"])
        A("dve", lambda e: e.tensor_copy(out=selcolb[:].rearrange("p a b -> p (a b)"), in_=selcolf[:]), ["selcolf"], ["selcolb"])
        A("dve", lambda e: e.tensor_copy(out=hselb[:], in_=hsel[:]), ["hsel"], ["hselb"])
        A("dve", lambda e: e.tensor_copy(out=sel16b[:], in_=sel16[:]), ["sel16"], ["sel16b"])
        A("dve", lambda e: e.tensor_copy(out=selT4b[:], in_=selT4[:]), ["selT4"], ["selT4b"])
        A("dve", lambda e: e.memset(onesb[:], 1.0), [], ["onesb"])
        A("dve", lambda e: e.memset(onesf[:], 1.0), [], ["onesf"])
        A("dve", lambda e: e.memset(v1_t[:], 1.0), [], ["v1"])

        castctr = [0]
        SFW = 3 * FW
        SBW = ABN // 2
        PW = min(SFW // 2, SBW, 2048)

        def cast_rows(src_ap, dst_ap, ncols, tag):
            i = castctr[0]
            castctr[0] += 1
            h = i % 2
            sbt = arena_b[:, h * SBW:h * SBW + ncols]
            eng = ("act", "dve")[i % 2]
            sft = arena_f[:, h * (SFW // 2):h * (SFW // 2) + ncols]
            A("sp", lambda e: e.dma_start(out=sft, in_=src_ap), writes=[("stf", h)], dma=True)
            if eng == "act":
                A("act", lambda e: e.activation(out=sbt, in_=sft, func=AF.Copy), [("stf", h)], [("stb", h)])
            else:
                A(eng, lambda e: e.tensor_copy(out=sbt, in_=sft), [("stf", h)], [("stb", h)])
            A("sp", lambda e: e.dma_start(out=dst_ap, in_=sbt), reads=[("stb", h)], writes=[tag], dma=True)

        def prepass(l):
            A("pool", lambda e: e.memset(arena_b[:, 0:2], 0.0), [], ARB + ARF + [("stb", 0), ("stb", 1), ("stf", 0), ("stf", 1)])
            wv = win_d[l].rearrange("(k p) n -> p k n", p=128)
            for k in range(8):
                for c0 in range(0, NC2, PW):
                    c1 = min(NC2, c0 + PW)
                    cast_rows(wv[:, k, c0:c1], winb_d[l][:, k, c0:c1], c1 - c0, ("winb", l))
            bv = wb_d[l].rearrange("(k p) n -> p k n", p=128)
            for k in range(16):
                cast_rows(bv[:, k, :], wbb_d[l][:, k, :], D, ("wbb", l))
            ov_ = wo_d[l].rearrange("(k p) n -> p k n", p=128)
            for k in range(8):
                cast_rows(ov_[:, k, :], wob_d[l][:, k, :], D, ("wob", l))
            A("pool", lambda e: e.memset(arena_b[:, 0:2], 0.0), [], ARB + ARF + [("stb", 0), ("stb", 1), ("stf", 0), ("stf", 1)])

        def layer_setup(l):
            load(cp[l][:], cp_d[l], ("cp", l))
            load(rp16[l][:], rp16_d[l], ("rp16", l))
            load(rp4[l][:], rp4_d[l], ("rp4", l))
            A("act", lambda e: e.activation(out=negA0[l][:], in_=rp16[l][:, 1:2], func=AF.Exp), [("rp16", l)], [("negA0", l)])
            A("dve", lambda e: e.tensor_scalar(out=negA[l][:], in0=negA0[l][:], scalar1=-1.0, scalar2=None, op0=ALU.mult),
              [("negA0", l)], [("negA", l)])
            A("dve", lambda e: e.tensor_scalar(out=nbf[l][:], in0=rp4[l][:, 1:2], scalar1=-1.0, scalar2=None, op0=ALU.mult),
              [("rp4", l)], [("nbf", l)])
            if l == 0:
                A("dve", lambda e: e.memset(lbc[l][:], 0.0), [], [("lbc", l)])
                A("dve", lambda e: e.memset(omlc[l][:], 1.0), [], [("omlc", l)])
            else:
                o0 = CP["hglb0"]
                o1 = CP["hglb1"]
                A("act", lambda e: e.activation(out=rs1[:], in_=cp[l][:, o0:o0 + 4], func=AF.Exp), [("cp", l)], ["rs1"])
                A("act", lambda e: e.activation(out=rs2[:], in_=cp[l][:, o1:o1 + 4], func=AF.Exp), [("cp", l)], ["rs2"])
                A("dve", lambda e: e.tensor_tensor(out=rs3[:], in0=rs1[:], in1=rs2[:], op=ALU.add), ["rs1", "rs2"], ["rs3"])
                A("dve", lambda e: e.reciprocal(out=rs4[:], in_=rs3[:]), ["rs3"], ["rs4"])
                A("dve", lambda e: e.tensor_tensor(out=lbc[l][:], in0=rs2[:], in1=rs4[:], op=ALU.mult), ["rs2", "rs4"], [("lbc", l)])
                A("dve", lambda e: e.tensor_tensor(out=omlc[l][:], in0=rs1[:], in1=rs4[:], op=ALU.mult), ["rs1", "rs4"], [("omlc", l)])
            A("sp", lambda e: e.dma_start(out=arena_f[:, 0:1536], in_=mlw_d[l]), writes=ARF, dma=True)
            A("dve", lambda e: e.tensor_copy(out=mlw[l][:].rearrange("p a b c -> p (a b c)"), in_=arena_f[:, 0:1536]), ARF, [("mlw", l)])
            A("sp", lambda e: e.dma_start(out=arena_f[:, 0:96], in_=wif_d[l]), writes=ARF, dma=True)
            sv = arena_f[:, 0:96].rearrange("p (h j c) -> p h j c", h=4, j=3)
            A("dve", lambda e: e.tensor_scalar(out=sv[:, :, 0, :], in0=sv[:, :, 0, :], scalar1=float(np.sqrt(128.0)), scalar2=None,
                                               op0=ALU.mult), ARF, ARF)
            A("dve", lambda e: e.tensor_copy(out=wif[l][:].rearrange("p a b -> p (a b)"), in_=arena_f[:, 0:96]), ARF, [("wif", l)])
            for t_, nm in [(haloA[l], "haloA"), (haloM[l], "haloM"), (Sssd[l], "Sssd"), (Cml[l], "Cml"),
                           (Shg[l], "Shg"), (Gprev[l], "Gprev"), (Mprev[l], "Mprev")]:
                A("pool", lambda e, t_=t_: e.memset(t_[:], 0.0), [], [(nm, l)])

        wctr = [0]

        def wload(src_ap, kc, ncols, deps):
            i = wctr[0] % NSLOT
            wctr[0] += 1
            dst = wslot[i][:, 0:kc, 0:ncols]
            A("sp", lambda e: e.dma_start(out=dst, in_=src_ap), reads=deps, writes=[("wslot", i)], dma=True)
            return wslot[i], ("wslot", i)

        evctr = [0]

        def evac_copy(out_ap, in_ap, reads, writes):
            eng = ("act", "dve")[evctr[0] % 2]
            evctr[0] += 1
            if eng == "act":
                A("act", lambda e: e.activation(out=out_ap, in_=in_ap, func=AF.Copy), reads, writes)
            else:
                A("dve", lambda e: e.tensor_copy(out=out_ap, in_=in_ap), reads, writes)

        def proj_cols(l, col0, ntiles, consume):
            t = 0
            while t < ntiles:
                n = min(6, ntiles - t)
                w, wres = wload(winb_d[l][:, :, col0 + t * 128:col0 + (t + n) * 128], 8, n * 128, [("winb", l)])
                for i in range(n):
                    ct = t + i
                    pb, pr = bank()
                    for k in range(8):
                        A("pe", lambda e, k=k, i=i, pb=pb, w=w: e.matmul(pb[:, 0:W], lhsT=w[:, k, i * 128:(i + 1) * 128], rhs=hnT[:, k, :],
                                                                         start=(k == 0), stop=(k == 7)), [wres, "hnT"], [pr])
                    consume(ct, pb, pr)
                t += n

        dctr = [0]

        def conv_tile(l, raw, ct_raw, dcol_base, ncolw, ct_idx, bias_col, out_ap, rtag, wtag):
            c = cp[l]
            di = dctr[0] % 2
            dctr[0] += 1
            for j in range(4):
                colw = dcol_base + j * ncolw + ct_idx
                if j % 2 == 0:
                    A("dve", lambda e, j=j, colw=colw: e.tensor_scalar(out=diag[:, di, j, :], in0=idf[:], scalar1=c[:, colw:colw + 1], scalar2=None, op0=ALU.mult),
                      ["idf", ("cp", l)], [("diag", di)])
                else:
                    A("act", lambda e, j=j, colw=colw: e.activation(out=diag[:, di, j, :], in_=idf[:], func=AF.Copy, scale=c[:, colw:colw + 1]),
                      ["idf", ("cp", l)], [("diag", di)])
            pb, pr = bank()
            for j in range(4):
                A("pe", lambda e, j=j, pb=pb: e.matmul(pb[:, 0:W], lhsT=diag[:, di, j, :], rhs=raw[:, ct_raw, j:j + W], start=(j == 0), stop=(j == 3)),
                  [("diag", di), rtag], [pr])
            A("act", lambda e, pb=pb: e.activation(out=out_ap, in_=pb[:, 0:W], func=AF.Silu, bias=c[:, bias_col:bias_col + 1]), [pr, ("cp", l)], [wtag])

        def rms_small(j):
            A("act", lambda e: e.activation(out=xsb[:], in_=xres[:, j, :], func=AF.Square, accum_out=ssq[:, j:j + 1]), [("xres", j)], ["xsb", "ssq"])
            A("dve", lambda e: e.tensor_scalar(out=rs1[:, j:j + 1], in0=ssq[:, j:j + 1], scalar1=1.0 / D, scalar2=EPS, op0=ALU.mult, op1=ALU.add), ["ssq"], ["rs1"])
            A("act", lambda e: e.activation(out=rs2[:, j:j + 1], in_=rs1[:, j:j + 1], func=AF.Ln), ["rs1"], ["rs2"])
            A("act", lambda e: e.activation(out=rs3[:, j:j + 1], in_=rs2[:, j:j + 1], func=AF.Exp, scale=-0.5), ["rs2"], ["rs3"])

        def layer(l, sc):
            c = cp[l]
            A("act", lambda e: e.activation(out=Sssdb[:], in_=Sssd[l][:], func=AF.Copy), [("Sssd", l)], ["Sssdb"])
            A("act", lambda e: e.activation(out=Cmlb[:], in_=Cml[l][:], func=AF.Copy), [("Cml", l)], ["Cmlb"])
            A("act", lambda e: e.activation(out=Shgb[:], in_=Shg[l][:], func=AF.Copy), [("Shg", l)], ["Shgb"])
            for j in range(NCH):
                rms_small(j)
                A("dve", lambda e, j=j: e.tensor_scalar(out=xsb[:], in0=xres[:, j, :], scalar1=rs3[:, j:j + 1], scalar2=None, op0=ALU.mult),
                  [("xres", j), "rs3"], ["xsb"])
                tb, tr = tbank()
                for k in range(8):
                    A("pe", lambda e, k=k, tb=tb: e.transpose(out=tb[:, k * 128:(k + 1) * 128], in_=xsb[:, k * 128:(k + 1) * 128], identity=idb[:]),
                      ["xsb", "idb"], [tr])
                nwv = c[:, CP["normw"]:CP["normw"] + 8].unsqueeze(2).to_broadcast([128, 8, 128])
                A("dve", lambda e, j=j, tb=tb, nwv=nwv: e.tensor_tensor(out=hnT[:, :, j * 128:(j + 1) * 128],
                                                                         in0=tb[:].rearrange("p (k t) -> p k t", k=8), in1=nwv, op=ALU.mult),
                  [tr, ("cp", l)], ["hnT"])

            rawA = view(arena_b, 0, [12, W + 4])
            convA = view(arena_b, oR1, [12, W])
            zs = view(arena_f, 0, [8, W])
            A("pool", lambda e: e.tensor_copy(out=rawA[:, :, 0:3], in_=haloA[l][:, :, 0:3]), [("haloA", l)], ["rawA"])

            def consA(ct, pb, pr):
                evac_copy(rawA[:, ct, 3:3 + W], pb[:, 0:W], [pr], ["rawA"])
            proj_cols(l, 0, 12, consA)
            A("pool", lambda e: e.tensor_copy(out=haloA[l][:, :, 0:3], in_=rawA[:, :, W:W + 3]), ["rawA"], [("haloA", l)])
            for ct in range(12):
                conv_tile(l, rawA, ct, CP["convwA"], 12, ct, CP["convbA"] + ct, convA[:, ct, :], "rawA", "convA")
            w, wr = wload(winb_d[l][:, :, 1536:1568], 8, 32, [("winb", l)])
            pb, pr = bank()
            for k in range(8):
                A("pe", lambda e, k=k, pb=pb, w=w: e.matmul(pb[0:16, 0:W], lhsT=w[:, k, 0:16], rhs=hnT[:, k, :], start=(k == 0), stop=(k == 7)), [wr, "hnT"], [pr])
            dt, la, cum, ncum, wend = r16a, r16b, r16c, r16d, r16e
            A("act", lambda e, pb=pb: e.activation(out=la[:], in_=pb[0:16, 0:W], func=AF.Exp, bias=rp16[l][:, 0:1]), [pr, ("rp16", l)], ["r16b"])
            A("act", lambda e: e.activation(out=dt[:], in_=la[:], func=AF.Ln, bias=1.0), ["r16b"], ["r16a"])
            A("dve", lambda e: e.tensor_scalar(out=wend[:], in0=dt[:], scalar1=negA[l][:], scalar2=None, op0=ALU.mult), ["r16a", ("negA", l)], ["r16e"])
            A("dve", lambda e: e.tensor_tensor_scan(out=cum[:], data0=rst128[:], data1=wend[:], initial=0.0, op0=ALU.mult, op1=ALU.add),
              ["r16e", "rst128"], ["r16c"])
            A("dve", lambda e: e.tensor_scalar(out=ncum[:], in0=cum[:], scalar1=-1.0, scalar2=None, op0=ALU.mult), ["r16c"], ["r16d"])
            A("act", lambda e: e.activation(out=ecumb[:], in_=cum[:], func=AF.Exp), ["r16c"], ["ecumb"])
            A("dve", lambda e: e.tensor_copy(out=cumhi[:], in_=cum[:]), ["r16c"], ["cumhi"])
            A("dve", lambda e: e.tensor_copy(out=cumhif[:], in_=cumhi[:]), ["cumhi"], ["cumhif"])
            A("dve", lambda e: e.tensor_tensor(out=cumlo[:], in0=cum[:], in1=cumhif[:], op=ALU.subtract), ["r16c", "cumhif"], ["cumlo"])
            A("dve", lambda e: e.tensor_scalar(out=ncumhi[:], in0=cumhi[:], scalar1=-1.0, scalar2=None, op0=ALU.mult), ["cumhi"], ["ncumhi"])
            A("dve", lambda e: e.tensor_scalar(out=ncumlo[:], in0=cumlo[:], scalar1=-1.0, scalar2=None, op0=ALU.mult), ["cumlo"], ["ncumlo"])
            for ch in range(NCH):
                A("act", lambda e, ch=ch: e.activation(out=la[:, ch * 128:(ch + 1) * 128], in_=cum[:, ch * 128:(ch + 1) * 128], func=AF.Exp,
                                                       scale=-1.0, bias=cum[:, ch * 128 + 127:ch * 128 + 128]), ["r16c"], ["r16b"])
            A("dve", lambda e: e.tensor_tensor(out=wend[:], in0=la[:], in1=dt[:], op=ALU.mult), ["r16b", "r16a"], ["r16e"])

            def consZ(ct, pb, pr):
                A("act", lambda e: e.activation(out=zs[:, ct, :], in_=pb[:, 0:W], func=AF.Silu), [pr], ["f0", "f1"])
            proj_cols(l, 1568, 8, consZ)
            for ch in range(NCH):
                t0 = ch * 128
                pb, pr = bank()
                A("pe", lambda e, pb=pb, t0=t0: e.transpose(out=pb[:, 0:16], in_=dt[:, t0:t0 + 128], identity=idf[0:16, 0:16]), ["r16a", "idf"], [pr])
                A("pe", lambda e, pb=pb, t0=t0: e.transpose(out=pb[:, 16:32], in_=wend[:, t0:t0 + 128], identity=idf[0:16, 0:16]), ["r16e", "idf"], [pr])
                A("dve", lambda e, pb=pb: e.tensor_copy(out=tok32[:], in_=pb[:, 0:32]), [pr], ["tok32"])
                A("act", lambda e, t0=t0: e.activation(out=r16g[:], in_=cum[:, t0 + 127:t0 + 128], func=AF.Exp), ["r16c"], ["r16g"])
                A("dve", lambda e: e.tensor_scalar(out=r16f[:], in0=idf[0:16, 0:16], scalar1=r16g[:], scalar2=None, op0=ALU.mult), ["r16g", "idf"], ["r16f"])
                pb2, pr2 = bank()
                A("pe", lambda e, pb2=pb2: e.matmul(pb2[:, 0:16], lhsT=onesf[0:16, 0:128], rhs=r16f[:], start=True, stop=True), ["onesf", "r16f"], [pr2])
                A("dve", lambda e, pb2=pb2: e.tensor_copy(out=decbc[:], in_=pb2[:, 0:16]), [pr2], ["decbc"])
                tb, tr = tbank()
                for ct in range(8):
                    A("pe", lambda e, ct=ct, tb=tb, t0=t0: e.transpose(out=tb[:, ct * 128:(ct + 1) * 128], in_=convA[:, ct, t0:t0 + 128], identity=idb[:]),
                      ["convA", "idb"], [tr])
                dtv = tok32[:, 0:16].unsqueeze(2).to_broadcast([128, 16, 64])
                dwv = tok32[:, 16:32].unsqueeze(2).to_broadcast([128, 16, 64])
                A("dve", lambda e, tb=tb, dtv=dtv: e.tensor_tensor(out=xdt_t[:].rearrange("p (h d) -> p h d", d=64),
                                                                    in0=tb[:].rearrange("p (h d) -> p h d", d=64), in1=dtv, op=ALU.mult), [tr, "tok32"], ["xdt"])
                A("dve", lambda e, tb=tb, dwv=dwv: e.tensor_tensor(out=xdtw_t[:].rearrange("p (h d) -> p h d", d=64),
                                                                    in0=tb[:].rearrange("p (h d) -> p h d", d=64), in1=dwv, op=ALU.mult), [tr, "tok32"], ["xdtw"])
                tb2, tr2 = tbank()
                for g in range(2):
                    A("pe", lambda e, g=g, tb2=tb2, t0=t0: e.transpose(out=tb2[:, g * 128:(g + 1) * 128], in_=convA[:, 8 + g, t0:t0 + 128], identity=idb[:]),
                      ["convA", "idb"], [tr2])
                A("act", lambda e, tb2=tb2: e.activation(out=btok_t[:], in_=tb2[:, 0:256], func=AF.Copy), [tr2], ["btok"])
                pcb, prcb = bank()
                for g in range(2):
                    A("pe", lambda e, g=g, pcb=pcb, t0=t0: e.matmul(pcb[:, g * 128:(g + 1) * 128], lhsT=convA[:, 8 + g, t0:t0 + 128],
                                                                     rhs=convA[:, 10 + g, t0:t0 + 128], start=True, stop=True), ["convA"], [prcb])
                for hq in range(4):
                    A("pool", lambda e, hq=hq, t0=t0: e.tensor_tensor(out=cdhi[:].rearrange("p (j l) -> p j l", j=4),
                                                                       in0=sel16b[:, hq, :].rearrange("p (j l) -> p j l", j=4),
                                                                       in1=cumhi[:, t0:t0 + 128].unsqueeze(1).to_broadcast([16, 4, 128]), op=ALU.mult),
                      ["sel16b", "cumhi"], ["cdhi"])
                    A("pool", lambda e, hq=hq, t0=t0: e.tensor_tensor(out=cdlo[:].rearrange("p (j l) -> p j l", j=4),
                                                                       in0=sel16b[:, hq, :].rearrange("p (j l) -> p j l", j=4),
                                                                       in1=cumlo[:, t0:t0 + 128].unsqueeze(1).to_broadcast([16, 4, 128]), op=ALU.mult),
                      ["sel16b", "cumlo"], ["cdlo"])
                    pd, prd = bank()
                    A("pe", lambda e, pd=pd, hq=hq, t0=t0: e.matmul(pd[:], lhsT=ncumhi[:, t0:t0 + 128], rhs=sel16b[:, hq, :], start=True, stop=False),
                      ["ncumhi", "sel16b"], [prd])
                    A("pe", lambda e, pd=pd, hq=hq, t0=t0: e.matmul(pd[:], lhsT=ncumlo[:, t0:t0 + 128], rhs=sel16b[:, hq, :], start=False, stop=False),
                      ["ncumlo", "sel16b"], [prd])
                    A("pe", lambda e, pd=pd: e.matmul(pd[:], lhsT=onesb[0:16, :], rhs=cdhi[:], start=False, stop=False), ["onesb", "cdhi"], [prd])
                    A("pe", lambda e, pd=pd: e.matmul(pd[:], lhsT=onesb[0:16, :], rhs=cdlo[:], start=False, stop=False), ["onesb", "cdlo"], [prd])
                    A("pe", lambda e, pd=pd: e.matmul(pd[:], lhsT=idb[:], rhs=mask4b[:], start=False, stop=True), ["idb", "mask4b"], [prd])
                    A("act", lambda e, pd=pd: e.activation(out=dec_t[:], in_=pd[:], func=AF.Exp), [prd], ["dec"])
                    g = hq // 2
                    A("dve", lambda e, hq=hq, g=g, pcb=pcb: e.tensor_tensor(out=sc_t[:, hq * 512:(hq + 1) * 512].rearrange("p (j l) -> p j l", j=4),
                                                                             in0=dec_t[:].rearrange("p (j l) -> p j l", j=4),
                                                                             in1=pcb[:, g * 128:(g + 1) * 128].unsqueeze(1).to_broadcast([128, 4, 128]),
                                                                             op=ALU.mult), ["dec", prcb], ["sc"])
                for half in range(2):
                    py1, pr1 = bank()
                    py2, pr2_ = bank()
                    pe_, pre = bank()
                    for cc in range(4):
                        ct = half * 4 + cc
                        g = ct // 4
                        for hh in range(2):
                            h = 2 * ct + hh
                            A("pe", lambda e, py1=py1, cc=cc, hh=hh, h=h: e.matmul(py1[hh * 64:(hh + 1) * 64, cc * 128:(cc + 1) * 128],
                                                                                  lhsT=xdt_t[:, h * 64:(h + 1) * 64], rhs=sc_t[:, h * 128:(h + 1) * 128],
                                                                                  start=True, stop=True), ["xdt", "sc"], [pr1])
                        A("pe", lambda e, py2=py2, cc=cc, ct=ct, g=g, t0=t0: e.matmul(py2[:, cc * 128:(cc + 1) * 128], lhsT=Sssdb[:, ct * 128:(ct + 1) * 128],
                                                                                      rhs=convA[:, 10 + g, t0:t0 + 128], start=True, stop=True), ["Sssdb", "convA"], [pr2_])
                        A("pe", lambda e, pe_=pe_, cc=cc, ct=ct, t0=t0: e.matmul(pe_[:, cc * 128:(cc + 1) * 128], lhsT=hselb[:, ct, :], rhs=ecumb[:, t0:t0 + 128],
                                                                                 start=True, stop=True), ["hselb", "ecumb"], [pre])
                    A("act", lambda e, pe_=pe_: e.activation(out=ebc_t[:], in_=pe_[:], func=AF.Copy), [pre], ["ebc"])
                    A("dve", lambda e, py2=py2: e.tensor_tensor(out=ytmp_t[:], in0=py2[:], in1=ebc_t[:], op=ALU.mult), [pr2_, "ebc"], ["ytmp"])
                    A("dve", lambda e, py1=py1: e.tensor_tensor(out=ytmp_t[:], in0=py1[:], in1=ytmp_t[:], op=ALU.add), [pr1, "ytmp"], ["ytmp"])
                    for cc in range(4):
                        ct = half * 4 + cc
                        col = CP["ssdD"] + ct
                        A("dve", lambda e, cc=cc, ct=ct, col=col, t0=t0: e.scalar_tensor_tensor(out=ytmp_t[:, cc * 128:(cc + 1) * 128], in0=convA[:, ct, t0:t0 + 128],
                                                                                                scalar=c[:, col:col + 1], in1=ytmp_t[:, cc * 128:(cc + 1) * 128],
                                                                                                op0=ALU.mult, op1=ALU.add), ["convA", "ytmp", ("cp", l)], ["ytmp"])
                    A("dve", lambda e, half=half, t0=t0: e.tensor_tensor(out=yg_t[:, half * 4:half * 4 + 4, t0:t0 + 128],
                                                                          in0=ytmp_t[:].rearrange("p (c t) -> p c t", c=4),
                                                                          in1=zs[:, half * 4:half * 4 + 4, t0:t0 + 128], op=ALU.mult), ["ytmp", "f0", "f1"], ["yg"])
                for g in range(2):
                    psl, prs = bank()
                    A("pe", lambda e, g=g, psl=psl: e.matmul(psl[:], lhsT=btok_t[:, g * 128:(g + 1) * 128], rhs=xdtw_t[:, g * 512:(g + 1) * 512],
                                                             start=True, stop=True), ["btok", "xdtw"], [prs])
                    dv = decbc[:, g * 8:(g + 1) * 8].unsqueeze(2).to_broadcast([128, 8, 64])
                    A("dve", lambda e, g=g, dv=dv: e.tensor_tensor(out=Sssd[l][:, g * 512:(g + 1) * 512].rearrange("p (h d) -> p h d", d=64),
                                                                    in0=Sssd[l][:, g * 512:(g + 1) * 512].rearrange("p (h d) -> p h d", d=64), in1=dv, op=ALU.mult),
                      [("Sssd", l), "decbc"], [("Sssd", l)])
                    A("dve", lambda e, g=g, psl=psl: e.tensor_tensor(out=Sssd[l][:, g * 512:(g + 1) * 512], in0=psl[:], in1=Sssd[l][:, g * 512:(g + 1) * 512],
                                                                      op=ALU.add), [prs, ("Sssd", l)], [("Sssd", l)])
                A("act", lambda e: e.activation(out=Sssdb[:], in_=Sssd[l][:], func=AF.Copy), [("Sssd", l)], ["Sssdb"])
            for g in range(2):
                pn, prn = bank()
                for cc in range(4):
                    ct = g * 4 + cc
                    A("act", lambda e, ct=ct, cc=cc: e.activation(out=sq_t[:, cc, :], in_=yg_t[:, ct, :], func=AF.Square), ["yg"], [("sq", cc)])
                    A("pe", lambda e, pn=pn, cc=cc: e.matmul(pn[:, 0:W], lhsT=onesb[:], rhs=sq_t[:, cc, :], start=(cc == 0), stop=(cc == 3)),
                      ["onesb", ("sq", cc)], [prn])
                A("act", lambda e, pn=pn: e.activation(out=nrm_t[:], in_=pn[:, 0:W], func=AF.Ln, scale=1.0 / 512, bias=EPS), [prn], ["nrm"])
                A("act", lambda e: e.activation(out=nrm2_t[:], in_=nrm_t[:], func=AF.Exp, scale=-0.5), ["nrm"], ["nrm2"])
                for cc in range(4):
                    ct = g * 4 + cc
                    col = CP["ssdnw"] + ct
                    A("dve", lambda e, ct=ct, col=col: e.scalar_tensor_tensor(out=yT[:, ct, :], in0=yg_t[:, ct, :], scalar=c[:, col:col + 1], in1=nrm2_t[:],
                                                                              op0=ALU.mult, op1=ALU.mult), ["yg", "nrm2", ("cp", l)], ["yg"])

            rawM = view(arena_b, 0, [4, W + 4])
            mconv = view(arena_b, 4 * (W + 4), [4, W])
            moT = view(arena_b, 4 * (W + 4) + 4 * W, [4, W])
            mzT = view(arena_b, oR1, [4, W])
            qT = view(arena_b, oR1 + 4 * W, [4, W])
            kT = view(arena_b, oR1 + 8 * W, [4, W])
            vT_t = view(arena_b, oR2, [4, W])
            A("pool", lambda e: e.tensor_copy(out=rawM[:, :, 0:3], in_=haloM[l][:, :, 0:3]), [("haloM", l)], ["rawA"])

            def consM(ct, pb, pr):
                if ct < 4:
                    evac_copy(rawM[:, ct, 3:3 + W], pb[:, 0:W], [pr], ["rawA"])
                elif ct < 8:
                    A("act", lambda e: e.activation(out=moT[:, ct - 4, :], in_=pb[:, 0:W], func=AF.Sigmoid), [pr], ["rawA"])
                else:
                    A("act", lambda e: e.activation(out=mzT[:, ct - 8, :], in_=pb[:, 0:W], func=AF.Silu), [pr], ["convA"])
            proj_cols(l, 2592, 12, consM)
            A("pool", lambda e: e.tensor_copy(out=haloM[l][:, :, 0:3], in_=rawM[:, :, W:W + 3]), ["rawA"], [("haloM", l)])
            for ct in range(4):
                conv_tile(l, rawM, ct, CP["convwM"], 4, ct, CP["convbM"] + ct, mconv[:, ct, :], "rawA", "rawA")
            qscale = float(128.0 ** -0.5)
            for h in range(4):
                pb, pr = bank()
                A("pe", lambda e, h=h, pb=pb: e.matmul(pb[:, 0:W], lhsT=mlw[l][:, 0, h, :], rhs=mconv[:, h, :], start=True, stop=True), [("mlw", l), "rawA"], [pr])
                A("act", lambda e, h=h, pb=pb: e.activation(out=qT[:, h, :], in_=pb[:, 0:W], func=AF.Copy, scale=qscale), [pr], ["convA"])
                pb, pr = bank()
                A("pe", lambda e, h=h, pb=pb: e.matmul(pb[:, 0:W], lhsT=mlw[l][:, 1, h, :], rhs=mconv[:, h, :], start=True, stop=True), [("mlw", l), "rawA"], [pr])
                A("dve", lambda e, h=h, pb=pb: e.tensor_copy(out=kT[:, h, :], in_=pb[:, 0:W]), [pr], ["convA"])
                pb, pr = bank()
                A("pe", lambda e, h=h, pb=pb: e.matmul(pb[:, 0:W], lhsT=mlw[l][:, 2, h, :], rhs=rawM[:, h, 3:3 + W], start=True, stop=True), [("mlw", l), "rawA"], [pr])
                A("act", lambda e, h=h, pb=pb: e.activation(out=vT_t[:, h, :], in_=pb[:, 0:W], func=AF.Copy), [pr], ["vT"])
            pi, pri = bank()
            pf, prf = bank()
            srcs = [qT, kT, vT_t]
            for idx in range(12):
                h, j = idx // 3, idx % 3
                A("pe", lambda e, idx=idx, h=h, j=j, pi=pi: e.matmul(pi[0:4, 0:W], lhsT=wif[l][:, idx, 0:4], rhs=srcs[j][:, h, :], start=(idx == 0), stop=(idx == 11)),
                  [("wif", l), "convA", "vT"], [pri])
            for idx in range(12):
                h, j = idx // 3, idx % 3
                A("pe", lambda e, idx=idx, h=h, j=j, pf=pf: e.matmul(pf[0:4, 0:W], lhsT=wif[l][:, idx, 4:8], rhs=srcs[j][:, h, :], start=(idx == 0), stop=(idx == 11)),
                  [("wif", l), "convA", "vT"], [prf])
            R4 = r4
            A("act", lambda e, pf=pf: e.activation(out=R4["e1"][:], in_=pf[0:4, 0:W], func=AF.Exp, scale=-1.0, bias=nbf[l][:]), [prf, ("nbf", l)], ["r4wint"])
            A("act", lambda e: e.activation(out=R4["lfn"][:], in_=R4["e1"][:], func=AF.Ln, bias=1.0), ["r4wint"], ["r4wend"])
            A("dve", lambda e: e.tensor_tensor_scan(out=R4["G"][:], data0=onesf[0:4, 0:W], data1=R4["lfn"][:], initial=Gprev[l][:],
                                                    op0=ALU.mult, op1=ALU.subtract), ["r4wend", "onesf", ("Gprev", l)], ["r4G"])
            A("dve", lambda e, pi=pi: e.scalar_tensor_tensor(out=R4["R"][:], in0=pi[0:4, 0:W], scalar=rp4[l][:, 0:1], in1=R4["G"][:], op0=ALU.add, op1=ALU.subtract),
              [pri, "r4G", ("rp4", l)], ["r4R"])
            A("dve", lambda e: e.tensor_tensor_scan(out=R4["Mt"][:], data0=onesf[0:4, 0:W], data1=R4["R"][:], initial=Mprev[l][:], op0=ALU.mult, op1=ALU.max),
              ["r4R", "onesf", ("Mprev", l)], ["r4Mt"])
            A("dve", lambda e: e.tensor_scalar(out=R4["nMt"][:], in0=R4["Mt"][:], scalar1=-1.0, scalar2=None, op0=ALU.mult), ["r4Mt"], ["r4nMt"])
            A("dve", lambda e: e.scalar_tensor_tensor(out=R4["t"][:], in0=R4["G"][:], scalar=-1.0, in1=R4["Mt"][:], op0=ALU.mult, op1=ALU.subtract),
              ["r4G", "r4Mt"], ["r4wint"])
            A("act", lambda e: e.activation(out=R4["emt"][:], in_=R4["t"][:], func=AF.Exp), ["r4wint"], ["r4emt"])
            for ch in range(NCH):
                t0 = ch * 128
                if ch == 0:
                    min_ap = Mprev[l][:]
                    mres = ("Mprev", l)
                else:
                    min_ap = R4["Mt"][:, t0 - 1:t0]
                    mres = "r4Mt"
                A("act", lambda e, t0=t0, min_ap=min_ap: e.activation(out=R4["wint"][:, t0:t0 + 128], in_=R4["Mt"][:, t0:t0 + 128], func=AF.Exp, scale=-1.0, bias=min_ap),
                  ["r4Mt", mres, "r4emt"], ["r4wint"])
                A("act", lambda e, t0=t0: e.activation(out=R4["wend"][:, t0:t0 + 128], in_=R4["R"][:, t0:t0 + 128], func=AF.Exp, bias=R4["nMt"][:, t0 + 127:t0 + 128]),
                  ["r4R", "r4nMt", "r4G"], ["r4wend"])
            for ch in range(NCH):
                A("dve", lambda e, ch=ch: e.tensor_scalar(out=r4s[:, ch * 4:(ch + 1) * 4], in0=idf[0:4, 0:4], scalar1=R4["wint"][:, ch * 128 + 127:ch * 128 + 128],
                                                          scalar2=None, op0=ALU.mult), ["idf", "r4wint"], ["r4s"])
            A("dve", lambda e: e.tensor_copy(out=wintb[:], in_=R4["wint"][:]), ["r4wint"], ["wintb"])
            pb, pr = bank()
            A("pe", lambda e, pb=pb: e.matmul(pb[:, 0:4 * NCH], lhsT=onesf[0:4, 0:128], rhs=r4s[:, 0:4 * NCH], start=True, stop=True), ["onesf", "r4s"], [pr])
            A("dve", lambda e, pb=pb: e.tensor_copy(out=aoldbc[:, 0:4 * NCH], in_=pb[:, 0:4 * NCH]), [pr], ["aoldbc"])
            A("pool", lambda e: e.tensor_copy(out=Gprev[l][:], in_=R4["G"][:, W - 1:W]), ["r4G"], [("Gprev", l)])
            A("pool", lambda e: e.tensor_copy(out=Mprev[l][:], in_=R4["Mt"][:, W - 1:W]), ["r4Mt", "r4wint"], [("Mprev", l)])
            hT = view(arena_f, 0, [4, W])
            for ch in range(NCH):
                t0 = ch * 128
                pb, pr = bank()
                A("pe", lambda e, pb=pb, t0=t0: e.transpose(out=pb[:, 0:4], in_=R4["wend"][:, t0:t0 + 128], identity=idf[0:4, 0:4]), ["r4wend", "idf"], [pr])
                A("dve", lambda e, pb=pb: e.tensor_copy(out=tok32[:, 0:4], in_=pb[:, 0:4]), [pr], ["tok32"])
                pk, prk = bank()
                pv, prv = bank()
                for h in range(4):
                    A("pe", lambda e, h=h, pk=pk, t0=t0: e.matmul(pk[:, h * 128:(h + 1) * 128], lhsT=mconv[:, h, t0:t0 + 128], rhs=mlw[l][:, 1, h, :], start=True, stop=True),
                      ["rawA", ("mlw", l)], [prk])
                    A("pe", lambda e, h=h, pv=pv, t0=t0: e.matmul(pv[:, h * 128:(h + 1) * 128], lhsT=rawM[:, h, 3 + t0:3 + t0 + 128], rhs=mlw[l][:, 2, h, :], start=True, stop=True),
                      ["rawA", ("mlw", l)], [prv])
                wv_ = tok32[:, 0:4].unsqueeze(2).to_broadcast([128, 4, 128])
                A("dve", lambda e, pk=pk, wv_=wv_: e.tensor_tensor(out=kw_t[:], in0=pk[:].rearrange("p (h e) -> p h e", h=4), in1=wv_, op=ALU.mult), [prk, "tok32"], ["kw"])
                A("act", lambda e, pv=pv: e.activation(out=v1_t[:, :, 0:128], in_=pv[:].rearrange("p (h e) -> p h e", h=4), func=AF.Copy), [prv], ["v1"])
                A("pool", lambda e, t0=t0: e.tensor_tensor(out=nmdiag[:].rearrange("p (j l) -> p j l", j=4), in0=sel4[:].rearrange("p (j l) -> p j l", j=4),
                                                           in1=R4["nMt"][:, t0:t0 + 128].unsqueeze(1).to_broadcast([4, 4, 128]), op=ALU.mult), ["sel4", "r4nMt"], ["nmdiag"])
                pd, prd = bank()
                A("pe", lambda e, pd=pd, t0=t0: e.matmul(pd[:], lhsT=R4["R"][:, t0:t0 + 128], rhs=sel4[:], start=True, stop=False), ["r4R", "sel4"], [prd])
                A("pe", lambda e, pd=pd: e.matmul(pd[:], lhsT=onesf[0:4, 0:128], rhs=nmdiag[:], start=False, stop=False), ["onesf", "nmdiag"], [prd])
                A("pe", lambda e, pd=pd: e.matmul(pd[:], lhsT=idb[:], rhs=mask4b[:], start=False, stop=True), ["idb", "mask4b"], [prd])
                A("act", lambda e, pd=pd: e.activation(out=dec_t[:], in_=pd[:], func=AF.Exp), [prd], ["dec"])
                pq, prq = bank()
                for h in range(4):
                    A("pe", lambda e, h=h, pq=pq, t0=t0: e.matmul(pq[:, h * 128:(h + 1) * 128], lhsT=kT[:, h, t0:t0 + 128], rhs=qT[:, h, t0:t0 + 128], start=True, stop=True),
                      ["convA"], [prq])
                A("dve", lambda e, pq=pq: e.tensor_tensor(out=sc_t[:, 0:512], in0=pq[:], in1=dec_t[:], op=ALU.mult), [prq, "dec"], ["sc"])
                pw, prw = bank()
                for h in range(4):
                    A("pe", lambda e, h=h, pw=pw, t0=t0: e.matmul(pw[:, h * 128:(h + 1) * 128], lhsT=selT4b[:, h, :], rhs=wintb[:, t0:t0 + 128], start=True, stop=True),
                      ["selT4b", "wintb"], [prw])
                A("dve", lambda e, pw=pw, t0=t0: e.tensor_tensor(out=qw_t[:].rearrange("p (h t) -> p h t", h=4), in0=qT[:, :, t0:t0 + 128],
                                                                  in1=pw[:].rearrange("p (h t) -> p h t", h=4), op=ALU.mult), ["convA", prw], ["qw"])
                pn, prn = bank()
                for h in range(4):
                    A("pe", lambda e, h=h, pn=pn: e.matmul(pn[:, h * 128:(h + 1) * 128], lhsT=v1_t[:, h, 0:128], rhs=sc_t[:, h * 128:(h + 1) * 128], start=True, stop=False),
                      ["v1", "sc"], [prn])
                    A("pe", lambda e, h=h, pn=pn: e.matmul(pn[:, h * 128:(h + 1) * 128], lhsT=Cmlb[:, h, 0:128], rhs=qw_t[:, h * 128:(h + 1) * 128], start=False, stop=True),
                      ["Cmlb", "qw"], [prn])
                A("dve", lambda e: e.tensor_copy(out=nsel[:], in_=selcolb[:]), ["selcolb"], ["nsel"])
                for h in range(4):
                    A("dve", lambda e, h=h: e.tensor_copy(out=nsel[:, h, h:h + 1], in_=Cmlb[:, h, 128:129]), ["Cmlb", "nsel"], ["nsel"])
                pdi, prdi = bank()
                for h in range(4):
                    A("pe", lambda e, h=h, pdi=pdi: e.matmul(pdi[0:4, 0:128], lhsT=selcolb[:, h, :], rhs=sc_t[:, h * 128:(h + 1) * 128], start=(h == 0), stop=(h == 3)),
                      ["selcolb", "sc"], [prdi])
                pde, prde = bank()
                for h in range(4):
                    A("pe", lambda e, h=h, pde=pde, t0=t0: e.matmul(pde[0:4, 0:128], lhsT=nsel[:, h, :], rhs=qT[:, h, t0:t0 + 128], start=(h == 0), stop=(h == 3)),
                      ["nsel", "convA"], [prde])
                A("dve", lambda e, pde=pde, t0=t0: e.tensor_tensor(out=r4den[:], in0=pde[0:4, 0:128], in1=R4["wint"][:, t0:t0 + 128], op=ALU.mult),
                  [prde, "r4wint"], ["r4den"])
                A("dve", lambda e, pdi=pdi: e.tensor_tensor(out=r4den2[:], in0=pdi[0:4, 0:128], in1=r4den[:], op=ALU.add), [prdi, "r4den"], ["r4den2"])
                A("dve", lambda e: e.tensor_scalar(out=r4den[:], in0=r4den2[:], scalar1=-1.0, scalar2=None, op0=ALU.mult), ["r4den2"], ["r4den"])
                A("dve", lambda e: e.tensor_tensor(out=r4den2[:], in0=r4den2[:], in1=r4den[:], op=ALU.max), ["r4den2", "r4den"], ["r4den2"])
                A("dve", lambda e, t0=t0: e.tensor_tensor(out=r4den[:], in0=r4den2[:], in1=R4["emt"][:, t0:t0 + 128], op=ALU.max),
                  ["r4den2", "r4emt"], ["r4den"])
                A("dve", lambda e: e.reciprocal(out=r4rden[:], in_=r4den[:]), ["r4den"], ["r4rden"])
                A("dve", lambda e: e.tensor_copy(out=rdenb[:], in_=r4rden[:]), ["r4rden"], ["rdenb"])
                pr_, prr = bank()
                for h in range(4):
                    A("pe", lambda e, h=h, pr_=pr_: e.matmul(pr_[:, h * 128:(h + 1) * 128], lhsT=selT4b[:, h, :], rhs=rdenb[:], start=True, stop=True),
                      ["selT4b", "rdenb"], [prr])
                A("act", lambda e, pr_=pr_: e.activation(out=ebc_t[:], in_=pr_[:], func=AF.Copy), [prr], ["ebc"])
                A("dve", lambda e, pn=pn, t0=t0: e.tensor_tensor(out=hT[:, :, t0:t0 + 128], in0=pn[:].rearrange("p (h t) -> p h t", h=4),
                                                                  in1=ebc_t[:].rearrange("p (h t) -> p h t", h=4), op=ALU.mult), [prn, "ebc"], ["f0"])
                for hp in range(2):
                    pc, prc = bank()
                    for hh in range(2):
                        h = hp * 2 + hh
                        A("pe", lambda e, h=h, hh=hh, pc=pc: e.matmul(pc[:, hh * 129:(hh + 1) * 129], lhsT=kw_t[:, h, :], rhs=v1_t[:, h, :], start=True, stop=True),
                          ["kw", "v1"], [prc])
                    av = aoldbc[:, ch * 4 + hp * 2:ch * 4 + hp * 2 + 2].unsqueeze(2).to_broadcast([128, 2, 129])
                    A("dve", lambda e, hp=hp, av=av: e.tensor_tensor(out=Cml[l][:, hp * 2:hp * 2 + 2, :], in0=Cml[l][:, hp * 2:hp * 2 + 2, :], in1=av, op=ALU.mult),
                      [("Cml", l), "aoldbc"], [("Cml", l)])
                    A("dve", lambda e, hp=hp, pc=pc: e.tensor_tensor(out=Cml[l][:, hp * 2:hp * 2 + 2, :], in0=pc[:, 0:258].rearrange("p (h e) -> p h e", h=2),
                                                                      in1=Cml[l][:, hp * 2:hp * 2 + 2, :], op=ALU.add), [prc, ("Cml", l)], [("Cml", l)])
                A("act", lambda e: e.activation(out=Cmlb[:], in_=Cml[l][:], func=AF.Copy), [("Cml", l)], ["Cmlb"])
            for h in range(4):
                pm, prm = bank()
                A("pe", lambda e, h=h, pm=pm: e.matmul(pm[:, 0:W], lhsT=onesf[:, 0:128], rhs=hT[:, h, :], start=True, stop=True), ["onesf", "f0"], [prm])
                A("dve", lambda e, h=h, pm=pm: e.scalar_tensor_tensor(out=hT[:, h, :], in0=pm[:, 0:W], scalar=-1.0 / 128, in1=hT[:, h, :], op0=ALU.mult, op1=ALU.add),
                  [prm, "f0"], ["f0"])
                A("act", lambda e, h=h: e.activation(out=sqf_t[:, 0:W], in_=hT[:, h, :], func=AF.Square), ["f0"], ["ytmp"])
                pv_, prv_ = bank()
                A("pe", lambda e, pv_=pv_: e.matmul(pv_[:, 0:W], lhsT=onesf[:, 0:128], rhs=sqf_t[:, 0:W], start=True, stop=True), ["onesf", "ytmp"], [prv_])
                A("act", lambda e, pv_=pv_: e.activation(out=nrm_t[:], in_=pv_[:, 0:W], func=AF.Ln, scale=1.0 / 128, bias=EPS), [prv_], ["nrm"])
                A("act", lambda e: e.activation(out=nrm2_t[:], in_=nrm_t[:], func=AF.Exp, scale=-0.5), ["nrm"], ["nrm2"])
                cn = CP["mlnw"] + h
                cs = CP["mlskip"] + h
                A("dve", lambda e, h=h, cn=cn: e.scalar_tensor_tensor(out=hT[:, h, :], in0=hT[:, h, :], scalar=c[:, cn:cn + 1], in1=nrm2_t[:], op0=ALU.mult, op1=ALU.mult),
                  ["f0", "nrm2", ("cp", l)], ["f0"])
                A("dve", lambda e, h=h: e.tensor_tensor(out=hT[:, h, :], in0=hT[:, h, :], in1=moT[:, h, :], op=ALU.mult), ["f0", "rawA"], ["f0"])
                A("dve", lambda e, h=h, cs=cs: e.scalar_tensor_tensor(out=hT[:, h, :], in0=mconv[:, h, :], scalar=c[:, cs:cs + 1], in1=hT[:, h, :], op0=ALU.mult, op1=ALU.add),
                  ["f0", "rawA", ("cp", l)], ["f0"])
                A("dve", lambda e, h=h: e.tensor_tensor(out=yT[:, 8 + h, :], in0=hT[:, h, :], in1=mzT[:, h, :], op=ALU.mult), ["f0", "convA"], [("yT", 8 + h)])

            qs = view(arena_b, 0, [4, W])
            gzs = view(arena_b, 4 * W, [4, W])
            qtl = view(arena_b, 8 * W, [4, W])
            ktl = view(arena_b, oR1, [4, W])
            kwl = view(arena_b, oR1 + 4 * W, [4, W])
            vTh = view(arena_b, oR1 + 8 * W, [4, W])
            sg = view(arena_f, 0, [4, W])
            tmpf = view(arena_f, FW, [4, W])
            cumh = view(arena_f, 2 * FW, [4, W])
            def consH(ct, pb, pr):
                if ct < 4:
                    A("act", lambda e: e.activation(out=qs[:, ct, :], in_=pb[:, 0:W], func=AF.Silu), [pr], ["rawA"])
                elif ct < 8:
                    h = ct - 4
                    A("act", lambda e: e.activation(out=sg[:, h, :], in_=pb[:, 0:W], func=AF.Sigmoid, scale=-1.0), [pr], ["f0"])
                    A("act", lambda e: e.activation(out=cumh[:, h, :], in_=pb[:, 0:W], func=AF.Sigmoid), [pr], ["f2"])
                    A("dve", lambda e: e.scalar_tensor_tensor(out=cumh[:, h, :], in0=sg[:, h, :], scalar=lbc[l][:, h:h + 1], in1=cumh[:, h, :], op0=ALU.mult, op1=ALU.add),
                      ["f0", "f2", ("lbc", l)], ["f2"])
                else:
                    A("act", lambda e: e.activation(out=gzs[:, ct - 8, :], in_=pb[:, 0:W], func=AF.Silu), [pr], ["rawA"])
            proj_cols(l, 4128, 12, consH)
            for h in range(4):
                A("act", lambda e, h=h: e.activation(out=tmpf[:, h, :], in_=cumh[:, h, :], func=AF.Ln), ["f2"], ["f1"])
            for h in range(4):
                A("dve", lambda e, h=h: e.tensor_tensor_scan(out=cumh[:, h, :], data0=rst64[:], data1=tmpf[:, h, :], initial=0.0, op0=ALU.mult, op1=ALU.add),
                  ["f1", "rst64"], ["f2"])

            def consV(ct, pb, pr):
                evac_copy(vTh[:, ct, :], pb[:, 0:W], [pr], ["convA"])
            proj_cols(l, 5664, 4, consV)
            for h in range(4):
                A("act", lambda e, h=h: e.activation(out=tmpf[:, h, :], in_=cumh[:, h, :], func=AF.Exp), ["f2"], ["f1"])
                A("dve", lambda e, h=h: e.tensor_tensor(out=qtl[:, h, :], in0=qs[:, h, :], in1=tmpf[:, h, :], op=ALU.mult), ["rawA", "f1"], ["rawA"])
                A("dve", lambda e, h=h: e.tensor_scalar(out=nrm_t[:], in0=cumh[:, h, :], scalar1=-80.0, scalar2=-1.0, op0=ALU.max, op1=ALU.mult), ["f2"], ["nrm"])
                A("act", lambda e, h=h: e.activation(out=tmpf[:, h, :], in_=nrm_t[:], func=AF.Exp), ["nrm", "rawA"], ["f1"])
                A("dve", lambda e, h=h: e.scalar_tensor_tensor(out=ktl[:, h, :], in0=sg[:, h, :], scalar=omlc[l][:, h:h + 1], in1=tmpf[:, h, :], op0=ALU.mult, op1=ALU.mult),
                  ["f0", "f1", ("omlc", l)], ["convA"])
                for b in range(NHB):
                    A("act", lambda e, h=h, b=b: e.activation(out=tmpf[:, h, b * 64:(b + 1) * 64], in_=cumh[:, h, b * 64:(b + 1) * 64], func=AF.Exp, scale=-1.0,
                                                              bias=cumh[:, h, b * 64 + 63:b * 64 + 64]), ["f2", "convA"], ["f1"])
                A("dve", lambda e, h=h: e.scalar_tensor_tensor(out=kwl[:, h, :], in0=sg[:, h, :], scalar=omlc[l][:, h:h + 1], in1=tmpf[:, h, :], op0=ALU.mult, op1=ALU.mult),
                  ["f0", "f1", ("omlc", l)], ["convA"])
            A("act", lambda e: e.activation(out=hdec_t[:], in_=cumh[:].rearrange("p h (b t) -> p h b t", t=64)[:, :, :, 63], func=AF.Exp), ["f2"], ["hdec"])
            oT = tmpf
            for b in range(NHB):
                s0 = b * 64
                tb, tr = tbank()
                tb2, tr2 = tbank()
                for h in range(4):
                    A("pe", lambda e, h=h, tb=tb, s0=s0: e.transpose(out=tb[0:64, h * 128:(h + 1) * 128], in_=kwl[:, h, s0:s0 + 64], identity=idb[:]), ["convA", "idb"], [tr])
                    A("pe", lambda e, h=h, tb2=tb2, s0=s0: e.transpose(out=tb2[0:64, h * 128:(h + 1) * 128], in_=vTh[:, h, s0:s0 + 64], identity=idb[:]), ["convA", "idb"], [tr2])
                A("dve", lambda e, tb=tb: e.tensor_copy(out=kwtok_t[:], in_=tb[0:64, 0:512]), [tr], ["kwtok"])
                A("act", lambda e, tb2=tb2: e.activation(out=vtok_t[:], in_=tb2[0:64, 0:512], func=AF.Copy), [tr2], ["vtok"])
                pa, pra = bank()
                for h in range(4):
                    A("pe", lambda e, h=h, pa=pa, s0=s0: e.matmul(pa[0:64, h * 64:(h + 1) * 64], lhsT=ktl[:, h, s0:s0 + 64], rhs=qtl[:, h, s0:s0 + 64], start=True, stop=True),
                      ["convA", "rawA"], [pra])
                A("dve", lambda e, pa=pa: e.tensor_tensor(out=attn_t[:], in0=pa[0:64, 0:256], in1=maskhg[:], op=ALU.mult), [pra, "maskhg"], ["attn"])
                po, pro = bank()
                for h in range(4):
                    A("pe", lambda e, h=h, po=po: e.matmul(po[:, h * 64:(h + 1) * 64], lhsT=vtok_t[:, h * 128:(h + 1) * 128], rhs=attn_t[:, h * 64:(h + 1) * 64], start=True, stop=False),
                      ["vtok", "attn"], [pro])
                    A("pe", lambda e, h=h, po=po, s0=s0: e.matmul(po[:, h * 64:(h + 1) * 64], lhsT=Shgb[:, h, :], rhs=qtl[:, h, s0:s0 + 64], start=False, stop=True),
                      ["Shgb", "rawA"], [pro])
                A("act", lambda e, po=po, s0=s0: e.activation(out=oT[:, :, s0:s0 + 64], in_=po[:, 0:256].rearrange("p (h t) -> p h t", h=4), func=AF.Copy), [pro], ["f1"])
                pS, prS = bank()
                for h in range(4):
                    A("pe", lambda e, h=h, pS=pS: e.matmul(pS[:, h * 128:(h + 1) * 128], lhsT=kwtok_t[:, h * 128:(h + 1) * 128], rhs=vtok_t[:, h * 128:(h + 1) * 128], start=True, stop=True),
                      ["kwtok", "vtok"], [prS])
                dv = hdec_t[:, :, b:b + 1].to_broadcast([128, 4, 128])
                A("dve", lambda e, dv=dv: e.tensor_tensor(out=Shg[l][:], in0=Shg[l][:], in1=dv, op=ALU.mult), [("Shg", l), "hdec"], [("Shg", l)])
                A("dve", lambda e, pS=pS: e.tensor_tensor(out=Shg[l][:], in0=pS[:].rearrange("p (h v) -> p h v", h=4), in1=Shg[l][:], op=ALU.add), [prS, ("Shg", l)], [("Shg", l)])
                A("act", lambda e: e.activation(out=Shgb[:], in_=Shg[l][:], func=AF.Copy), [("Shg", l)], ["Shgb"])
            for h in range(4):
                A("act", lambda e, h=h: e.activation(out=sq_t[:, h, :], in_=oT[:, h, :], func=AF.Square), ["f1"], [("sq", h)])
                pn, prn = bank()
                A("pe", lambda e, h=h, pn=pn: e.matmul(pn[:, 0:W], lhsT=onesb[:], rhs=sq_t[:, h, :], start=True, stop=True), ["onesb", ("sq", h)], [prn])
                A("act", lambda e, pn=pn: e.activation(out=nrm_t[:], in_=pn[:, 0:W], func=AF.Ln, scale=1.0 / 128, bias=EPS), [prn], ["nrm"])
                A("act", lambda e: e.activation(out=nrm2_t[:], in_=nrm_t[:], func=AF.Exp, scale=-0.5), ["nrm"], ["nrm2"])
                cn = CP["hgnw"] + h
                A("dve", lambda e, h=h, cn=cn: e.scalar_tensor_tensor(out=oT[:, h, :], in0=oT[:, h, :], scalar=c[:, cn:cn + 1], in1=nrm2_t[:], op0=ALU.mult, op1=ALU.mult),
                  ["f1", "nrm2", ("cp", l)], ["f1"])
                A("dve", lambda e, h=h: e.tensor_tensor(out=yT[:, 12 + h, :], in0=oT[:, h, :], in1=gzs[:, h, :], op=ALU.mult), ["f1", "rawA"], [("yT", 12 + h)])

            gat = view(arena_b, 0, [24, W])
            def consG(ct, pb, pr):
                A("act", lambda e: e.activation(out=gat[:, ct, :], in_=pb[:, 0:W], func=AF.Sigmoid), [pr], ["rawA", "convA"])
            proj_cols(l, 6176, 24, consG)
            mt = view(arena_f, 0, [2, W])
            GR = ["rawA", "convA"]
            for cq in range(8):
                if cq % 4 == 0:
                    hb = cq // 4
                    w1, wr1 = wload(wbb_d[l][:, 0:8, hb * 512:(hb + 1) * 512], 8, 512, [("wbb", l)])
                    w2, wr2 = wload(wbb_d[l][:, 8:16, hb * 512:(hb + 1) * 512], 8, 512, [("wbb", l)])
                cq4 = cq % 4
                p0, pr0 = bank()
                p1, pr1 = bank()
                p2, pr2 = bank()
                for k in range(8):
                    A("pe", lambda e, k=k, cq4=cq4, p0=p0, w1=w1: e.matmul(p0[:, 0:W], lhsT=w1[:, k, cq4 * 128:(cq4 + 1) * 128], rhs=yT[:, k, :], start=(k == 0), stop=(k == 7)),
                      [wr1, "yg"], [pr0])
                for k in range(4):
                    A("pe", lambda e, k=k, cq4=cq4, p1=p1, w2=w2: e.matmul(p1[:, 0:W], lhsT=w2[:, k, cq4 * 128:(cq4 + 1) * 128], rhs=yT[:, 8 + k, :], start=(k == 0), stop=(k == 3)),
                      [wr2, ("yT", 8 + k)], [pr1])
                for k in range(4):
                    A("pe", lambda e, k=k, cq4=cq4, p2=p2, w2=w2: e.matmul(p2[:, 0:W], lhsT=w2[:, 4 + k, cq4 * 128:(cq4 + 1) * 128], rhs=yT[:, 12 + k, :], start=(k == 0), stop=(k == 3)),
                      [wr2, ("yT", 12 + k)], [pr2])
                A("dve", lambda e, cq=cq, p0=p0: e.tensor_tensor(out=mt[:, 0, :], in0=p0[:, 0:W], in1=gat[:, cq, :], op=ALU.mult), [pr0] + GR, ["f0"])
                A("dve", lambda e, cq=cq, p1=p1: e.tensor_tensor(out=mt[:, 1, :], in0=p1[:, 0:W], in1=gat[:, 8 + cq, :], op=ALU.mult), [pr1, "f0"] + GR, ["f0b"])
                A("dve", lambda e: e.tensor_tensor(out=mt[:, 0, :], in0=mt[:, 0, :], in1=mt[:, 1, :], op=ALU.add), ["f0", "f0b"], ["f0"])
                A("dve", lambda e, cq=cq, p2=p2: e.tensor_tensor(out=mt[:, 1, :], in0=p2[:, 0:W], in1=gat[:, 16 + cq, :], op=ALU.mult), [pr2, "f0"] + GR, ["f0b"])
                A("dve", lambda e, cq=cq: e.tensor_tensor(out=mergedT[:, cq, :], in0=mt[:, 0, :], in1=mt[:, 1, :], op=ALU.add), ["f0", "f0b"], ["hnT"])
            for n in range(2):
                w, wr = wload(wob_d[l][:, :, n * 512:(n + 1) * 512], 8, 512, [("wob", l)])
                for j in range(NCH):
                    pb, pr = bank()
                    for k in range(8):
                        A("pe", lambda e, k=k, j=j, pb=pb, w=w: e.matmul(pb[:], lhsT=mergedT[:, k, j * 128:(j + 1) * 128], rhs=w[:, k, 0:512],
                                                                         start=(k == 0), stop=(k == 7)), ["hnT", wr], [pr])
                    A("dve", lambda e, j=j, n=n, pb=pb: e.tensor_tensor(out=xres[:, j, n * 512:(n + 1) * 512], in0=pb[:], in1=xres[:, j, n * 512:(n + 1) * 512], op=ALU.add),
                      [pr, ("xres", j)], [("xres", j)])

        for l in range(depth):
            layer_setup(l)
        prepass(0)
        xv = x_d.rearrange("(s j p) d -> s p j d", p=128, j=NCH)
        ov = out_d.rearrange("(s j p) d -> s p j d", p=128, j=NCH)
        fnw_v = arena_f[:, 2 * FW:2 * FW + D]
        outs = []
        for sc in range(nsc):
            for j in range(NCH):
                A("act", lambda e, sc=sc, j=j: e.dma_start(out=xres[:, j, :], in_=xv[sc, :, j, :]), writes=[("xres", j)], dma=True)
            for l in range(depth):
                if sc == 0 and l >= 1:
                    prepass(l)
                layer(l, sc)
            A("act", lambda e: e.dma_start(out=fnw_v, in_=fnw_d), writes=["f2"], dma=True)
            for j in range(NCH):
                rms_small(j)
                A("dve", lambda e, j=j: e.scalar_tensor_tensor(out=xres[:, j, :], in0=xres[:, j, :], scalar=rs3[:, j:j + 1], in1=fnw_v, op0=ALU.mult, op1=ALU.mult),
                  [("xres", j), "rs3", "f2"], [("xres", j)])
                A("act", lambda e, sc=sc, j=j: e.dma_start(out=ov[sc, :, j, :], in_=xres[:, j, :]), reads=[("xres", j)], writes=[("out", sc, j)], dma=True)
                outs.append(("out", sc, j))
        A("sp", lambda e: e.nop(), reads=outs)

        sems = {e: es.enter_context(nc.semaphore("sem_" + e)) for e in S.ENGS}
        dsems = [es.enter_context(nc.semaphore(f"dsem{i}")) for i in range(S.n_dma_sems)]
        S.finalize()
        print("ops:", len(S.ops))
        S.emit(nc, sems, dsems)
    return nc


def host_consts():
    cst = {}
    cst["ident"] = np.eye(128, dtype=np.float32)
    s = np.arange(128)[:, None]
    lq = np.arange(128)[None, :]
    m = np.where(lq >= s, 0.0, NEG).astype(np.float32)
    cst["mask4"] = np.tile(m, (1, 4))
    s6 = np.arange(64)[:, None]
    l6 = np.arange(64)[None, :]
    cst["maskhg"] = np.tile((l6 >= s6).astype(np.float32), (1, 4))
    sel16 = np.zeros((16, 4, 4, 128), np.float32)
    for hq in range(4):
        for j in range(4):
            sel16[4 * hq + j, hq, j, :] = 1.0
    cst["sel16"] = sel16.reshape(16, 2048)
    sel4 = np.zeros((4, 4, 128), np.float32)
    for h in range(4):
        sel4[h, h, :] = 1.0
    cst["sel4"] = sel4.reshape(4, 512)
    hsel = np.zeros((16, 8, 128), np.float32)
    for ct in range(8):
        for m_ in range(128):
            hsel[2 * ct + m_ // 64, ct, m_] = 1.0
    cst["hsel"] = hsel.reshape(16, 1024)
    selT4 = np.zeros((4, 4, 128), np.float32)
    for h in range(4):
        selT4[h, h, :] = 1.0
    cst["selT4"] = selT4.reshape(4, 512)
    r = np.ones((16, 512), np.float32)
    r[:, 0::128] = 0.0
    cst["rst128"] = r
    r = np.ones((128, 512), np.float32)
    r[:, 0::HC] = 0.0
    cst["rst64"] = r
    selcol = np.zeros((128, 4, 4), np.float32)
    for h in range(4):
        selcol[:, h, h] = 1.0
    cst["selcol"] = selcol.reshape(128, 16)
    return cst


def col(v, n):
    return np.ascontiguousarray(np.asarray(v, np.float32).reshape(n, 128).T)


def host_layer_inputs(l, I):
    f = np.float32
    w_in = np.asarray(I["w_in"][l], f)
    o = {}
    win = np.zeros((D, NC2), f)
    src = [(0, 1536, cA), (1552, 2576, cZ), (1536, 1552, cDT), (2576, 3088, cMX), (3088, 3600, cMO), (3600, 4112, cMZ),
           (4112, 4624, cGQ), (4624, 5136, cGF), (5648, 6160, cGZ), (5136, 5648, cGI), (6160, 9232, cGATE)]
    for a, b, d in src:
        win[:, d:d + (b - a)] = w_in[:, a:b]
    o[f"win{l}"] = win
    o[f"wb{l}"] = np.ascontiguousarray(np.concatenate([I["w_branch_ssd"][l], I["w_branch_ml"][l], I["w_branch_hg"][l]], 0).astype(f))
    o[f"wo{l}"] = np.ascontiguousarray(np.asarray(I["w_out"][l], f))
    mlw = np.stack([I["ml_wq"][l], I["ml_wk"][l], I["ml_wv"][l]], 0).astype(f)
    o[f"mlw{l}"] = np.ascontiguousarray(mlw.transpose(2, 0, 1, 3).reshape(128, 1536))
    wif = np.asarray(I["ml_w_if"][l], f).reshape(4, 3, 128, 8)
    o[f"wif{l}"] = np.ascontiguousarray(wif.transpose(2, 0, 1, 3).reshape(128, 96))
    cpm = np.zeros((128, NCP), f)
    cpm[:, CP["normw"]:CP["normw"] + 8] = col(I["norm_w"][l], 8)
    cpm[:, CP["convbA"]:CP["convbA"] + 12] = col(I["ssd_conv_b"][l], 12)
    for j in range(4):
        cpm[:, CP["convwA"] + j * 12:CP["convwA"] + (j + 1) * 12] = col(I["ssd_conv_w"][l][j], 12)
        cpm[:, CP["convwM"] + j * 4:CP["convwM"] + (j + 1) * 4] = col(I["ml_conv_w"][l][j], 4)
    cpm[:, CP["convbM"]:CP["convbM"] + 4] = col(I["ml_conv_b"][l], 4)
    cpm[:, CP["ssdD"]:CP["ssdD"] + 8] = col(np.repeat(np.asarray(I["ssd_d"][l], f), 64), 8)
    cpm[:, CP["ssdnw"]:CP["ssdnw"] + 8] = col(I["ssd_norm_w"][l], 8)
    cpm[:, CP["mlnw"]:CP["mlnw"] + 4] = col(I["ml_norm_w"][l], 4)
    cpm[:, CP["mlskip"]:CP["mlskip"] + 4] = col(I["ml_skip"][l], 4)
    cpm[:, CP["hgnw"]:CP["hgnw"] + 4] = col(I["hg_norm_w"][l], 4)
    cpm[:, CP["hglb0"]:CP["hglb0"] + 4] = col(I["hg_lower_bounds"][0], 4)
    cpm[:, CP["hglb1"]:CP["hglb1"] + 4] = col(I["hg_lower_bounds"][1], 4)
    o[f"cp{l}"] = cpm
    o[f"rp16_{l}"] = np.ascontiguousarray(np.stack([I["ssd_dt_bias"][l], I["ssd_a_log"][l]], 1).astype(f))
    bif = np.asarray(I["ml_b_if"][l], f)
    o[f"rp4_{l}"] = np.ascontiguousarray(np.stack([bif[0:4], bif[4:8]], 1))
    return o


_CACHE = {}


def kernel(**inputs):
    I = {k: np.asarray(v) for k, v in inputs.items()}
    x = np.asarray(I["x"], np.float32)
    shared = dict(host_consts())
    for l in range(DEPTH):
        shared.update(host_layer_inputs(l, I))
    shared["fnw"] = np.ascontiguousarray(np.broadcast_to(np.asarray(I["final_norm_w"], np.float32)[None, :], (128, D)))
    if "nc" not in _CACHE:
        _CACHE["nc"] = build_program()
    nc = _CACHE["nc"]
    in_maps = []
    for c in range(8):
        m = dict(shared)
        m["x"] = np.ascontiguousarray(x[c % 4])
        in_maps.append(m)
    res = run_bass_kernel_spmd(nc, in_maps, core_ids=list(range(8)))
    out = np.stack([np.asarray(res.results[b]["out"], np.float32) for b in range(4)], 0)
    return out
```
